# Optimizing a Trainium2 kernel written in Bass

```python
import jax, jax.numpy as jnp
from jax import lax
import numpy as np

D_MODEL = 1024
BATCH = 8
SEQ = 8192
DEPTH = 1

D_MIX = D_MODEL
D_MLSTM = D_MIX // 2
D_MOBA = D_MIX - D_MLSTM
MLSTM_HEADS = 4
MLSTM_HEAD_DIM = D_MLSTM // MLSTM_HEADS
MLSTM_CHUNK = 64
CONV_WIDTH = 4
MOBA_HEADS = 8
MOBA_HEAD_DIM = D_MOBA // MOBA_HEADS
MOBA_BLOCK = 256
MOBA_TOPK = 3
MOBA_Q_CHUNK = 32
N_GROUPS = 4
EXPERTS_PER_GROUP = 8
N_EXPERTS = N_GROUPS * EXPERTS_PER_GROUP
EXPERT_TOPK = 2
D_EXPERT = 512
MOE_ROW_BLOCK = 256
EPS = 1e-6
IN_COLS = 4 * D_MLSTM + 2 * MLSTM_HEADS + 3 * D_MOBA

kernel_name = 'hymba_mlstm_moba_hmoe_adaln'


def rms_norm(x, g):
    xf = x.astype(jnp.float32)
    y = xf * lax.rsqrt(jnp.mean(xf * xf, axis=-1, keepdims=True) + EPS)
    return (y * g.astype(jnp.float32)).astype(x.dtype)


def split_heads(a, n_heads):
    b, s, _ = a.shape
    return a.reshape(b, s, n_heads, -1).transpose(0, 2, 1, 3)


def merge_heads(a):
    b, h, s, d = a.shape
    return a.transpose(0, 2, 1, 3).reshape(b, s, h * d)


def causal_depthwise_conv(x, w, b):
    s = x.shape[1]
    xp = jnp.pad(x, ((0, 0), (CONV_WIDTH - 1, 0), (0, 0)))
    y = b
    for j in range(CONV_WIDTH):
        y = y + w[j] * xp[:, j:j + s]
    return y


def mlstm_chunkwise(q, k, v, i_pre, f_pre):
    b, h, s, d = q.shape
    dv = v.shape[-1]
    nc = s // MLSTM_CHUNK
    out_dtype = v.dtype
    qf = q.astype(jnp.float32) * (d ** -0.5)
    kf = k.astype(jnp.float32)
    vf = v.astype(jnp.float32)
    log_i = i_pre.astype(jnp.float32)
    log_f = jax.nn.log_sigmoid(f_pre.astype(jnp.float32))

    def to_chunks(a):
        a = a.reshape(b, h, nc, MLSTM_CHUNK, *a.shape[3:])
        return jnp.moveaxis(a, 2, 0)

    causal = jnp.tril(jnp.ones((MLSTM_CHUNK, MLSTM_CHUNK), dtype=bool))

    def step(carry, inp):
        C, n, m = carry
        qt, kt, vt, it, ft = inp
        cum_f = jnp.cumsum(ft, axis=-1)
        cum_last = cum_f[..., -1]
        dmat = cum_f[..., :, None] - cum_f[..., None, :] + it[..., None, :]
        dmat = jnp.where(causal, dmat, -jnp.inf)
        inter = cum_f + m[..., None]
        m_t = jnp.maximum(inter, jnp.max(dmat, axis=-1))
        w_inter = jnp.exp(inter - m_t)
        sc = jnp.einsum('bhtd,bhsd->bhts', qt, kt) * jnp.exp(dmat - m_t[..., None])
        num = w_inter[..., None] * jnp.einsum('bhtd,bhde->bhte', qt, C) + jnp.einsum('bhts,bhse->bhte', sc, vt)
        den = w_inter * jnp.einsum('bhtd,bhd->bht', qt, n) + jnp.sum(sc, axis=-1)
        h_t = num / jnp.maximum(jnp.abs(den), jnp.exp(-m_t))[..., None]
        g_end = cum_last[..., None] - cum_f + it
        m_new = jnp.maximum(cum_last + m, jnp.max(g_end, axis=-1))
        w_old = jnp.exp(cum_last + m - m_new)
        w_new = jnp.exp(g_end - m_new[..., None])
        C_new = w_old[..., None, None] * C + jnp.einsum('bhs,bhsd,bhse->bhde', w_new, kt, vt)
        n_new = w_old[..., None] * n + jnp.einsum('bhs,bhsd->bhd', w_new, kt)
        return (C_new, n_new, m_new), h_t

    init = (jnp.zeros((b, h, d, dv), jnp.float32), jnp.zeros((b, h, d), jnp.float32),
            jnp.zeros((b, h), jnp.float32))
    _, hs = lax.scan(step, init, (to_chunks(qf), to_chunks(kf), to_chunks(vf),
                                  to_chunks(log_i), to_chunks(log_f)))
    return jnp.moveaxis(hs, 0, 2).reshape(b, h, s, dv).astype(out_dtype)


def moba_attention(q, k, v):
    b, h, s, d = q.shape
    nb = -(-s // MOBA_BLOCK)
    pad = nb * MOBA_BLOCK - s
    kp = jnp.pad(k.astype(jnp.float32), ((0, 0), (0, 0), (0, pad), (0, 0)))
    vp = jnp.pad(v.astype(jnp.float32), ((0, 0), (0, 0), (0, pad), (0, 0)))
    kb = kp.reshape(b, h, nb, MOBA_BLOCK, d)
    vb = vp.reshape(b, h, nb, MOBA_BLOCK, d)
    k_mean = jnp.mean(kb, axis=3)
    scale = d ** -0.5
    n_sel = min(MOBA_TOPK, nb - 1)
    bi = jnp.arange(b)[:, None, None, None]
    hi = jnp.arange(h)[None, :, None, None]
    blk_ids = jnp.arange(nb)

    def chunk(ci):
        t0 = ci * MOBA_Q_CHUNK
        qc = lax.dynamic_slice_in_dim(q, t0, MOBA_Q_CHUNK, axis=2).astype(jnp.float32)
        q_pos = t0 + jnp.arange(MOBA_Q_CHUNK)
        own = t0 // MOBA_BLOCK
        k_own = lax.dynamic_index_in_dim(kb, own, axis=2, keepdims=False)
        v_own = lax.dynamic_index_in_dim(vb, own, axis=2, keepdims=False)
        own_pos = own * MOBA_BLOCK + jnp.arange(MOBA_BLOCK)
        s_own = jnp.einsum('bhqd,bhkd->bhqk', qc, k_own) * scale
        s_own = jnp.where(own_pos[None, :] <= q_pos[:, None], s_own, -jnp.inf)
        if n_sel > 0:
            gate = jnp.einsum('bhqd,bhnd->bhqn', qc, k_mean)
            gate = jnp.where(blk_ids < own, gate, -jnp.inf)
            _, sel = lax.top_k(gate, n_sel)
            valid = sel < own
            k_sel = kb[bi, hi, sel]
            v_sel = vb[bi, hi, sel]
            s_sel = jnp.einsum('bhqd,bhqskd->bhqsk', qc, k_sel) * scale
            s_sel = jnp.where(valid[..., None], s_sel, -jnp.inf)
            p = jax.nn.softmax(jnp.concatenate(
                [s_sel.reshape(b, h, MOBA_Q_CHUNK, n_sel * MOBA_BLOCK), s_own], axis=-1), axis=-1)
            p_sel = p[..., :n_sel * MOBA_BLOCK].reshape(b, h, MOBA_Q_CHUNK, n_sel, MOBA_BLOCK)
            p_own = p[..., n_sel * MOBA_BLOCK:]
            return (jnp.einsum('bhqsk,bhqskd->bhqd', p_sel, v_sel)
                    + jnp.einsum('bhqk,bhkd->bhqd', p_own, v_own))
        p_own = jax.nn.softmax(s_own, axis=-1)
        return jnp.einsum('bhqk,bhkd->bhqd', p_own, v_own)

    outs = lax.map(chunk, jnp.arange(s // MOBA_Q_CHUNK))
    return jnp.moveaxis(outs, 0, 2).reshape(b, h, s, d).astype(q.dtype)


def hierarchical_moe(hx, w_rg, b_rg, w_re, b_re, w_gate, w_up, w_down):
    b, s, dm = hx.shape
    t = b * s
    xt = hx.reshape(t, dm)
    g_prob = jax.nn.softmax((xt @ w_rg).astype(jnp.float32) + b_rg, axis=-1)
    g_w, g_idx = lax.top_k(g_prob, 1)
    e_logits = ((xt @ w_re).astype(jnp.float32) + b_re).reshape(t, N_GROUPS, EXPERTS_PER_GROUP)
    e_logits = jnp.take_along_axis(e_logits, g_idx[:, :, None], axis=1)[:, 0]
    e_top, e_loc = lax.top_k(e_logits, EXPERT_TOPK)
    e_w = jax.nn.softmax(e_top, axis=-1) * g_w
    e_id = g_idx * EXPERTS_PER_GROUP + e_loc

    n_assign = t * EXPERT_TOPK
    flat_e = e_id.reshape(n_assign)
    flat_tok = jnp.repeat(jnp.arange(t), EXPERT_TOPK)
    flat_w = e_w.reshape(n_assign)
    order = jnp.argsort(flat_e)
    se, stok, sw = flat_e[order], flat_tok[order], flat_w[order]
    counts = jnp.zeros((N_EXPERTS,), jnp.int32).at[flat_e].add(1)
    starts = jnp.cumsum(counts) - counts
    padded = (counts + MOE_ROW_BLOCK - 1) // MOE_ROW_BLOCK * MOE_ROW_BLOCK
    pad_ends = jnp.cumsum(padded)
    pad_starts = pad_ends - padded
    dest = pad_starts[se] + (jnp.arange(n_assign) - starts[se])
    n_blocks = -(-n_assign // MOE_ROW_BLOCK) + N_EXPERTS
    rows = n_blocks * MOE_ROW_BLOCK
    x_pad = jnp.zeros((rows, dm), hx.dtype).at[dest].set(xt[stok])
    blk_expert = jnp.minimum(
        jnp.searchsorted(pad_ends, jnp.arange(n_blocks) * MOE_ROW_BLOCK, side='right'), N_EXPERTS - 1)

    def run_block(args):
        xb, e = args
        return (jax.nn.silu(xb @ w_gate[e]) * (xb @ w_up[e])) @ w_down[e]

    y_pad = lax.map(run_block, (x_pad.reshape(n_blocks, MOE_ROW_BLOCK, dm), blk_expert))
    y_sorted = y_pad.reshape(rows, dm)[dest].astype(jnp.float32)
    out = jnp.zeros((t, dm), jnp.float32).at[stok].add(y_sorted * sw[:, None])
    return out.reshape(b, s, dm).astype(hx.dtype)


def setup_inputs(seed: int = 0) -> dict:
    key = jax.random.key(seed)
    ks = jax.random.split(key, 24)
    f32 = jnp.float32

    def nrm(k, shape, sc):
        return sc * jax.random.normal(k, shape, f32)

    L = DEPTH
    x = nrm(ks[0], (BATCH, SEQ, D_MODEL), 1.0)
    c = nrm(ks[1], (BATCH, D_MODEL), 1.0)
    w_ada = nrm(ks[2], (L, D_MODEL, 6 * D_MODEL), 0.5 * D_MODEL ** -0.5)
    b_ada = nrm(ks[3], (L, 6 * D_MODEL), 0.02)
    g_norm1 = 1.0 + nrm(ks[4], (L, D_MODEL), 0.02)
    w_in = nrm(ks[5], (L, D_MODEL, IN_COLS), D_MODEL ** -0.5)
    w_conv = nrm(ks[6], (L, CONV_WIDTH, 2 * D_MLSTM), CONV_WIDTH ** -0.5)
    b_conv = nrm(ks[7], (L, 2 * D_MLSTM), 0.02)
    b_i = nrm(ks[8], (L, MLSTM_HEADS), 0.1)
    b_f = jnp.linspace(3.0, 6.0, MLSTM_HEADS, dtype=f32) + nrm(ks[9], (L, MLSTM_HEADS), 0.1)
    b_gates = jnp.concatenate([b_i, b_f], axis=-1)
    g_mlstm_head = 1.0 + nrm(ks[10], (L, D_MLSTM), 0.02)
    w_out = nrm(ks[11], (L, D_MIX, D_MODEL), D_MIX ** -0.5)
    g_norm2 = 1.0 + nrm(ks[12], (L, D_MODEL), 0.02)
    w_router_group = nrm(ks[13], (L, D_MODEL, N_GROUPS), D_MODEL ** -0.5)
    b_router_group = nrm(ks[14], (L, N_GROUPS), 0.01)
    w_router_expert = nrm(ks[15], (L, D_MODEL, N_EXPERTS), D_MODEL ** -0.5)
    b_router_expert = nrm(ks[16], (L, N_EXPERTS), 0.01)
    w_expert_gate = nrm(ks[17], (L, N_EXPERTS, D_MODEL, D_EXPERT), D_MODEL ** -0.5)
    w_expert_up = nrm(ks[18], (L, N_EXPERTS, D_MODEL, D_EXPERT), D_MODEL ** -0.5)
    w_expert_down = nrm(ks[19], (L, N_EXPERTS, D_EXPERT, D_MODEL), D_EXPERT ** -0.5)
    g_final = 1.0 + nrm(ks[20], (D_MODEL,), 0.02)
    return {'x': x, 'c': c, 'w_ada': w_ada, 'b_ada': b_ada, 'g_norm1': g_norm1, 'w_in': w_in,
            'w_conv': w_conv, 'b_conv': b_conv, 'b_gates': b_gates, 'g_mlstm_head': g_mlstm_head,
            'w_out': w_out, 'g_norm2': g_norm2, 'w_router_group': w_router_group,
            'b_router_group': b_router_group, 'w_router_expert': w_router_expert,
            'b_router_expert': b_router_expert, 'w_expert_gate': w_expert_gate,
            'w_expert_up': w_expert_up, 'w_expert_down': w_expert_down, 'g_final': g_final}


def reference(x, c, w_ada, b_ada, g_norm1, w_in, w_conv, b_conv, b_gates, g_mlstm_head,
              w_out, g_norm2, w_router_group, b_router_group, w_router_expert,
              b_router_expert, w_expert_gate, w_expert_up, w_expert_down, g_final):
    split_at = np.cumsum([D_MLSTM, D_MLSTM, D_MLSTM, D_MLSTM, 2 * MLSTM_HEADS, D_MOBA, D_MOBA]).tolist()
    for l in range(DEPTH):
        mod = jax.nn.silu(c) @ w_ada[l] + b_ada[l]
        shift1, scale1, gate1, shift2, scale2, gate2 = [m[:, None, :] for m in jnp.split(mod, 6, axis=-1)]

        hn = rms_norm(x, g_norm1[l]) * (1.0 + scale1) + shift1
        proj = hn @ w_in[l]
        q_m, k_m, v_m, o_m, gates, q_a, k_a, v_a = jnp.split(proj, split_at, axis=-1)

        qk = jax.nn.silu(causal_depthwise_conv(jnp.concatenate([q_m, k_m], axis=-1), w_conv[l], b_conv[l]))
        q_m, k_m = jnp.split(qk, 2, axis=-1)
        gates = gates + b_gates[l]
        i_pre = gates[..., :MLSTM_HEADS].transpose(0, 2, 1)
        f_pre = gates[..., MLSTM_HEADS:].transpose(0, 2, 1)
        h_m = mlstm_chunkwise(split_heads(q_m, MLSTM_HEADS), split_heads(k_m, MLSTM_HEADS),
                              split_heads(v_m, MLSTM_HEADS), i_pre, f_pre)
        h_m = rms_norm(h_m, g_mlstm_head[l].reshape(MLSTM_HEADS, 1, MLSTM_HEAD_DIM))
        h_m = merge_heads(h_m) * jax.nn.sigmoid(o_m)

        h_a = merge_heads(moba_attention(split_heads(q_a, MOBA_HEADS), split_heads(k_a, MOBA_HEADS),
                                         split_heads(v_a, MOBA_HEADS)))

        mix = jnp.concatenate([h_m, h_a], axis=-1) @ w_out[l]
        x = x + gate1 * mix

        hn2 = rms_norm(x, g_norm2[l]) * (1.0 + scale2) + shift2
        ffn = hierarchical_moe(hn2, w_router_group[l], b_router_group[l], w_router_expert[l],
                               b_router_expert[l], w_expert_gate[l], w_expert_up[l], w_expert_down[l])
        x = x + gate2 * ffn
    return rms_norm(x, g_final)
```

```python
import os
import numpy as np
from contextlib import ExitStack
import concourse.bass as bass
import concourse.mybir as mybir
from concourse.bass_utils import run_bass_kernel_spmd

F32 = mybir.dt.float32
BF16 = mybir.dt.bfloat16
I32 = mybir.dt.int32
AF = mybir.ActivationFunctionType
ALU = mybir.AluOpType
AX = mybir.AxisListType

D = 1024
INC = 3592
EPS = 1e-6
NEXP = 32
DEXP = 512
BIG = 1.0e30


class Tok:
    __slots__ = ("name", "writes", "xwrites", "reads", "sem", "total")

    def __init__(self, name=""):
        self.name = name
        self.writes = {}
        self.xwrites = {}
        self.reads = {}
        self.sem = None
        self.total = 0


class KB:
    def __init__(self, nc, stack):
        self.nc = nc
        self.stack = stack
        self.eng = {"pe": nc.tensor, "dve": nc.vector, "act": nc.scalar, "pool": nc.gpsimd, "sp": nc.sync}
        self.sems = {}
        self.cnt = {}
        self.semobj = {}
        for e in self.eng:
            s = stack.enter_context(nc.semaphore("s_" + e))
            self.sems[e] = s
            self.semobj[("e", e)] = s
            self.cnt[e] = 0
        self.waited = {e: {} for e in self.eng}
        self.ntok = 0
        self.dtoks = []

    def tok(self, name=""):
        return Tok(name)

    def _toksem(self, t, kind):
        if t.sem is None:
            t.sem = {}
            t.total = {}
            self.dtoks.append(t)
        if kind not in t.sem:
            self.ntok += 1
            t.sem[kind] = self.stack.enter_context(self.nc.semaphore("d%d" % self.ntok))
            t.total[kind] = 0
            self.semobj[("t", id(t), kind)] = t.sem[kind]
        return ("t", id(t), kind)

    def _wait(self, e, deps):
        w = self.waited[e]
        for k, v in deps.items():
            if k == ("e", "pe") and e == "pe":
                continue
            if w.get(k, 0) >= v:
                continue
            self.eng[e].wait_ge(self.semobj[k], v)
            w[k] = v

    @staticmethod
    def _merge(d, src):
        for k, v in src.items():
            if d.get(k, 0) < v:
                d[k] = v

    def op(self, e, fn, reads=(), writes=(), rw=(), pw=()):
        deps = {}
        for t in list(reads) + list(rw):
            self._merge(deps, t.writes)
        for t in list(writes) + list(rw):
            self._merge(deps, t.writes)
            self._merge(deps, t.reads)
        for t in pw:
            self._merge(deps, t.xwrites)
            self._merge(deps, t.reads)
        self._wait(e, deps)
        ins = fn(self.eng[e])
        self.cnt[e] += 1
        v = self.cnt[e]
        ins.then_inc(self.sems[e], 1)
        k = ("e", e)
        for t in reads:
            t.reads[k] = v
        for t in list(writes) + list(rw):
            t.writes[k] = v
            t.xwrites[k] = v
        for t in pw:
            t.writes[k] = v
        return ins

    def dma(self, q, out, in_, dst, src=None, store=False, fn=None, extra=(), waw=False):
        deps = {}
        for t in ([src] if src is not None else []) + list(extra):
            for k, v in t.writes.items():
                if deps.get(k, 0) < v:
                    deps[k] = v
        for k, v in dst.reads.items():
            if deps.get(k, 0) < v:
                deps[k] = v
        for k, v in dst.writes.items():
            if (waw or k[0] == "e") and deps.get(k, 0) < v:
                deps[k] = v
        self._wait(q, deps)
        kind = "sw" if q == "pool" else "hw"
        own = src if store else dst
        key = self._toksem(own, kind)
        if fn is None:
            ins = self.eng[q].dma_start(out=out, in_=in_)
        else:
            ins = fn(self.eng[q])
        own.total[kind] += 16
        ins.then_inc(own.sem[kind], 16)
        dst.writes[key] = own.total[kind]
        dst.xwrites[key] = own.total[kind]
        for t in ([src] if src is not None else []) + list(extra):
            t.reads[key] = own.total[kind]
        return ins

    def wait_tok(self, e, t):
        self._wait(e, dict(t.writes))

    def barrier(self):
        deps = {("e", e): self.cnt[e] for e in self.eng if self.cnt[e] > 0}
        for t in self.dtoks:
            for kind, tot in t.total.items():
                deps[("t", id(t), kind)] = tot
        for e in self.eng:
            d = {k: v for k, v in deps.items() if k != ("e", e)}
            self._wait(e, d)


def build(S, dbg=False, stop=99):
    nc = bass.Bass("TRN2", target_bir_lowering=False)
    NT = S // 512
    NB = S // 256
    NCH = S // 64
    NS = S // 128
    assert NCH <= 128

    def din(name, shape, dt=F32):
        return nc.dram_tensor(name, shape, dt, kind="ExternalInput").ap()

    skind = "ExternalOutput" if dbg else "Internal"

    def dscr(name, shape, dt):
        return nc.dram_tensor(name, shape, dt, kind=skind).ap()

    x = din("x", [S, D])
    c_l = din("c_l", [128, 8])
    w_ada = din("w_ada", [D, 6 * D])
    b_ada = din("b_ada", [1, 6 * D])
    g1_l = din("g1_l", [128, 8])
    g2_l = din("g2_l", [128, 8])
    gfin = din("gfin", [1, D])
    w_in = din("w_in", [D, INC])
    wconv_l = din("wconv_l", [128, 8, 4])
    bconv_l = din("bconv_l", [128, 8])
    bgi = din("bgi", [4, 1])
    bgf = din("bgf", [4, 1])
    gml = din("gml", [1, 512])
    w_out = din("w_out", [D, D])
    w_r = din("w_r", [D, 36])
    b_r = din("b_r", [1, 36])
    w_eg = din("w_eg", [NEXP * 2 * 128, 2048])
    w_eu = din("w_eu", [NEXP * 2 * 128, 2048])
    w_ed = din("w_ed", [NEXP * 2 * 128, 2048])
    NBLK = (2 * S) // 512 + NEXP
    trs_d = din("trs", [128, 128])
    thr_d = din("thr", [128, NBLK])
    hp_d = din("hp", [128, 2])
    ident_d = din("ident", [128, 128])
    tri_d = din("tri", [128, 128])
    sel4_d = din("sel4", [4, 4, 128])
    out = nc.dram_tensor("out", [S, D], F32, kind="ExternalOutput").ap()

    FM = dscr("FM", [16, 128, S], BF16)
    TM = dscr("TM", [S, 1536], BF16)
    G = dscr("G", [8, S], F32)
    HC = dscr("HC", [S, D], BF16)
    X1 = dscr("X1", [S, D], F32)
    XN = dscr("XN", [S, D], BF16)
    XP = dscr("XP", [NBLK * 512, D], BF16)
    YP = dscr("YP", [NBLK * 512, D], F32)

    with ExitStack() as st:
        kb = KB(nc, st)
        T = kb.tok

        def sbuf(ctx, name, shape, dt):
            return ctx.enter_context(nc.sbuf_tensor(name, shape, dt))

        def psum(ctx, name, shape, dt):
            return ctx.enter_context(nc.psum_tensor(name, shape, dt))

        ident_f = sbuf(st, "ident_f", [128, 128], F32)
        ident_b = sbuf(st, "ident_b", [128, 128], BF16)
        tri_b = sbuf(st, "tri_b", [128, 128], BF16)
        ones_f = sbuf(st, "ones_f", [128, 128], F32)
        a1 = sbuf(st, "a1", [128, 8], F32)
        b1 = sbuf(st, "b1", [128, 8], F32)
        a2 = sbuf(st, "a2", [128, 8], F32)
        b2 = sbuf(st, "b2", [128, 8], F32)
        gate1B = sbuf(st, "gate1B", [128, D], F32)
        gate2B = sbuf(st, "gate2B", [128, D], F32)
        OH1a = sbuf(st, "OH1a", [128, NS, NEXP], F32)
        OH2a = sbuf(st, "OH2a", [128, NS, NEXP], F32)
        w1a = sbuf(st, "w1a", [128, NS], F32)
        w2a = sbuf(st, "w2a", [128, NS], F32)
        dest1i = sbuf(st, "dest1i", [128, NS], I32)
        dest2i = sbuf(st, "dest2i", [128, NS], I32)
        idxw = sbuf(st, "idxw", [128, NBLK, 2], I32)
        t_const = T("const")
        t_mod = T("mod")
        t_cw = T("cw")
        t_FM, t_TM, t_G, t_HC, t_X1, t_XN, t_out = T("FM"), T("TM"), T("G"), T("HC"), T("X1"), T("XN"), T("out")
        t_XP, t_YP, t_rt = T("XP"), T("YP"), T("route")

        kb.dma("sp", ident_f[:], ident_d[:], t_const)
        kb.dma("pool", ident_b[:], ident_d[:], t_const)
        kb.dma("pool", tri_b[:], tri_d[:], t_const)
        t_ones = T("ones")
        kb.op("pool", lambda e: e.memset(ones_f[:], 1.0), writes=[t_ones])

        ph0A = ExitStack()
        w_in_sb = sbuf(ph0A, "w_in_sb", [128, 8, INC], BF16)
        t_w = T()
        w_in_v = w_in.rearrange("(kc p) n -> p kc n", p=128)
        for hf in range(2):
            kb.dma("pool", w_in_sb[:, :, hf * 1796:(hf + 1) * 1796], w_in_v[:, :, hf * 1796:(hf + 1) * 1796], t_w)

        with ExitStack() as ph:
            c_sb = sbuf(ph, "c_sb", [128, 8], F32)
            sc = sbuf(ph, "sc", [128, 8], F32)
            g1s = sbuf(ph, "g1s", [128, 8], F32)
            g2s = sbuf(ph, "g2s", [128, 8], F32)
            modrow = sbuf(ph, "modrow", [1, 6 * D], F32)
            brow = sbuf(ph, "brow", [1, 6 * D], F32)
            modT = sbuf(ph, "modT", [128, 48], F32)
            wa = [sbuf(ph, "wa%d" % i, [128, 8, 512], F32) for i in range(2)]
            ps0 = psum(ph, "ps0", [128, 512], F32)
            ps1 = psum(ph, "ps1", [128, 512], F32)
            t_c, t_sc, t_g, t_brow, t_modrow = T(), T(), T(), T(), T()
            t_wa = [T(), T()]
            t_ps0, t_ps1, t_modT = T(), T(), T()
            kb.dma("sp", c_sb[:], c_l[:], t_c)
            kb.dma("sp", g1s[:], g1_l[:], t_g)
            kb.dma("sp", g2s[:], g2_l[:], t_g)
            kb.dma("sp", brow[:], b_ada[:], t_brow)
            kb.op("act", lambda e: e.activation(sc[:], c_sb[:], AF.Silu), reads=[t_c], writes=[t_sc])
            wav = w_ada.rearrange("(kc p) n -> p kc n", p=128)
            for blk in range(12):
                bf = blk % 2
                kb.dma("sp", wa[bf][:], wav[:, :, blk * 512:(blk + 1) * 512], t_wa[bf])
                for kc in range(8):
                    kb.op("pe", lambda e, kc=kc, bf=bf: e.matmul(ps0[0:1, :], sc[:, kc:kc + 1], wa[bf][:, kc, :],
                                                               start=(kc == 0), stop=(kc == 7)),
                          reads=[t_sc, t_wa[bf]], writes=[t_ps0])
                kb.op("dve", lambda e, blk=blk: e.tensor_tensor(modrow[0:1, blk * 512:(blk + 1) * 512], ps0[0:1, :],
                                                                brow[0:1, blk * 512:(blk + 1) * 512], ALU.add),
                      reads=[t_ps0, t_brow], writes=[t_modrow])
            for (gB, c0) in ((gate1B, 2 * D), (gate2B, 5 * D)):
                for hf in range(2):
                    kb.op("pe", lambda e, c0=c0, hf=hf: e.matmul(ps1[:, :], ones_f[0:1, :], modrow[0:1, c0 + hf * 512:c0 + (hf + 1) * 512],
                                                                 start=True, stop=True),
                          reads=[t_modrow, t_ones], writes=[t_ps1])
                    kb.op("act", lambda e, gB=gB, hf=hf: e.activation(gB[:, hf * 512:(hf + 1) * 512], ps1[:, :], AF.Identity),
                          reads=[t_ps1], writes=[t_mod])
            for j in range(48):
                kb.op("pe", lambda e, j=j: e.matmul(ps0[:, j:j + 1], modrow[0:1, j * 128:(j + 1) * 128], ones_f[0:1, 0:1],
                                                    start=True, stop=True),
                      reads=[t_modrow, t_ones], writes=[t_ps0])
            kb.op("dve", lambda e: e.tensor_copy(modT[:], ps0[:, 0:48]), reads=[t_ps0], writes=[t_modT])
            kb.op("dve", lambda e: e.scalar_tensor_tensor(a1[:], modT[:, 8:16], 1.0, g1s[:], ALU.add, ALU.mult),
                  reads=[t_modT, t_g], writes=[t_mod])
            kb.op("dve", lambda e: e.tensor_copy(b1[:], modT[:, 0:8]), reads=[t_modT], writes=[t_mod])
            kb.op("dve", lambda e: e.scalar_tensor_tensor(a2[:], modT[:, 32:40], 1.0, g2s[:], ALU.add, ALU.mult),
                  reads=[t_modT, t_g], writes=[t_mod])
            kb.op("dve", lambda e: e.tensor_copy(b2[:], modT[:, 24:32]), reads=[t_modT], writes=[t_mod])
            kb.barrier()
        if stop <= 0:
            return nc

        with ExitStack() as ph:
            wc = sbuf(ph, "wc", [128, 8, 4], F32)
            bc = sbuf(ph, "bc", [128, 8], F32)
            bi_s = sbuf(ph, "bi_s", [4, 1], F32)
            bf_s = sbuf(ph, "bf_s", [4, 1], F32)
            xt = [sbuf(ph, "xt%d" % i, [128, 4, D], F32) for i in range(2)]
            junk = sbuf(ph, "junk", [128, D], BF16)
            ssq = sbuf(ph, "ssq", [128, 4], F32)
            rs = sbuf(ph, "rs", [128, 4], F32)
            xn2 = [sbuf(ph, "xn%d" % i, [128, 4, D], BF16) for i in range(2)]
            hnT = [sbuf(ph, "hnT%d" % i, [128, 8, 512], BF16) for i in range(2)]
            convbuf = sbuf(ph, "convbuf", [128, 8, 515], F32)
            cacc = [sbuf(ph, "cacc%d" % i, [128, 512], F32) for i in range(2)]
            sgt = [sbuf(ph, "sgt%d" % i, [128, 512], F32) for i in range(2)]
            stage_m = sbuf(ph, "stage_m", [128, 8, 512], BF16)
            stage_a = sbuf(ph, "stage_a", [128, 8, 512], BF16)
            stage_t = sbuf(ph, "stage_t", [128, 4, 1536], BF16)
            gst = sbuf(ph, "gst", [4, 2, 512], F32)
            pT = [psum(ph, "pT%d" % i, [128, 1024], BF16) for i in range(2)]
            pm = [psum(ph, "pm%d" % i, [128, 512], F32) for i in range(4)]
            pg = [psum(ph, "pg%d" % i, [4, 512], F32) for i in range(2)]
            t_wc = T()
            t_xt = [T(), T()]
            t_junk, t_ssq, t_rs, t_xn2 = T(), T(), T(), [T(), T()]
            t_hnT = [T(), T()]
            t_pT = [T(), T()]
            t_pm = [T() for _ in range(4)]
            t_pg = [T(), T()]
            t_conv = [T() for _ in range(8)]
            t_cacc = [T(), T()]
            t_sgt = [T(), T()]
            t_sm, t_sa, t_stt, t_gst = T(), T(), T(), T()
            kb.dma("sp", wc[:], wconv_l[:], t_wc)
            kb.dma("sp", bc[:], bconv_l[:], t_wc)
            kb.dma("sp", bi_s[:], bgi[:], t_wc)
            kb.dma("sp", bf_s[:], bgf[:], t_wc)
            kb.op("dve", lambda e: e.memset(convbuf[:, :, 0:3], 0.0), writes=t_conv)
            xv = x.rearrange("(n s p) d -> n p s d", s=4, p=128)
            FMv = FM.rearrange("g p t -> p g t")
            TMv = TM.rearrange("(n s p) c -> n p s c", s=4, p=128)
            kb.dma("sp", xt[0][:], xv[0], t_xt[0])
            pmi = [0]

            def next_pm():
                i = pmi[0] % 4
                pmi[0] += 1
                return i

            def normA(i):
                cur = i % 2
                xc = xt[cur]
                for s in range(4):
                    kb.op("act", lambda e, s=s, xc=xc: e.activation(junk[:], xc[:, s, :], AF.Square, accum_out=ssq[:, s:s + 1]),
                          reads=[t_xt[cur]], writes=[t_junk, t_ssq])
                kb.op("act", lambda e: e.activation(rs[:], ssq[:], AF.Sqrt, scale=1.0 / D, bias=EPS), reads=[t_ssq], writes=[t_rs])
                kb.op("dve", lambda e: e.reciprocal(rs[:], rs[:]), rw=[t_rs])
                for s in range(4):
                    kb.op("dve", lambda e, s=s, xc=xc, cur=cur: e.tensor_scalar(xn2[cur][:, s, :], xc[:, s, :], rs[:, s:s + 1], None, ALU.mult),
                          reads=[t_xt[cur], t_rs], pw=[t_xn2[cur]])

            normA(0)
            for i in range(NT):
                cur = i % 2
                if i + 1 < NT:
                    kb.dma("sp", xt[1 - cur][:], xv[i + 1], t_xt[1 - cur])
                xn, t_xn = xn2[cur], t_xn2[cur]
                hc = hnT[cur]
                for kc in range(8):
                    pb = kc % 2
                    for s in range(4):
                        kb.op("pe", lambda e, kc=kc, s=s, pb=pb, xn=xn: e.transpose(pT[pb][:, s * 128:(s + 1) * 128],
                                                                              xn[:, s, kc * 128:(kc + 1) * 128], ident_b[:]),
                              reads=[t_xn, t_const], writes=[t_pT[pb]])
                    if kc % 2 == 0:
                        kb.op("act", lambda e, kc=kc, pb=pb: e.activation(hc[:, kc, :], pT[pb][:, 0:512], AF.Identity,
                                                                          scale=a1[:, kc:kc + 1], bias=b1[:, kc:kc + 1]),
                              reads=[t_pT[pb], t_mod], pw=[t_hnT[cur]])
                    else:
                        kb.op("dve", lambda e, kc=kc, pb=pb: e.tensor_scalar(hc[:, kc, :], pT[pb][:, 0:512], a1[:, kc:kc + 1],
                                                                             b1[:, kc:kc + 1], ALU.mult, ALU.add),
                              reads=[t_pT[pb], t_mod], pw=[t_hnT[cur]])
                if i + 1 < NT:
                    normA(i + 1)
                for g in range(0, 8):
                    if g < 8:
                        c0 = g * 128
                    elif g < 12:
                        c0 = 2056 + (g - 8) * 128
                    else:
                        c0 = 2568 + (g - 12) * 128
                    pi = next_pm()
                    for kc in range(8):
                        kb.op("pe", lambda e, kc=kc, c0=c0, pi=pi: e.matmul(pm[pi][:, :], w_in_sb[:, kc, c0:c0 + 128], hc[:, kc, :],
                                                                          start=(kc == 0), stop=(kc == 7)),
                              reads=[t_w, t_hnT[cur]], writes=[t_pm[pi]])
                    if g < 8:
                        kb.op("act", lambda e, g=g, pi=pi: e.activation(convbuf[:, g, 3:515], pm[pi][:, :], AF.Identity),
                              reads=[t_pm[pi]], writes=[t_conv[g]])
                    elif g < 12:
                        kb.op("act", lambda e, g=g, pi=pi: e.activation(stage_a[:, g - 8, :], pm[pi][:, :], AF.Identity, scale=0.125),
                              reads=[t_pm[pi]], pw=[t_sa])
                    else:
                        kb.op("act", lambda e, g=g, pi=pi: e.activation(stage_a[:, g - 8, :], pm[pi][:, :], AF.Identity),
                              reads=[t_pm[pi]], pw=[t_sa])
                for g in range(8 if 'conv' not in os.environ.get('KSKIP', '') else 0):
                    cb = g % 2
                    kb.op("dve", lambda e, g=g, cb=cb: e.tensor_scalar(cacc[cb][:], convbuf[:, g, 0:512], wc[:, g, 0:1], bc[:, g:g + 1],
                                                                       ALU.mult, ALU.add),
                          reads=[t_conv[g], t_wc], writes=[t_cacc[cb]])
                    for j in range(1, 4):
                        kb.op("dve", lambda e, g=g, cb=cb, j=j: e.scalar_tensor_tensor(cacc[cb][:], convbuf[:, g, j:j + 512], wc[:, g, j:j + 1],
                                                                                      cacc[cb][:], ALU.mult, ALU.add),
                              reads=[t_conv[g], t_wc], rw=[t_cacc[cb]])
                    kb.op("act", lambda e, cb=cb: e.activation(sgt[cb][:], cacc[cb][:], AF.Sigmoid), reads=[t_cacc[cb]], writes=[t_sgt[cb]])
                    qs = (128.0 ** -0.5) if g < 4 else 1.0
                    kb.op("dve", lambda e, g=g, cb=cb, qs=qs: e.scalar_tensor_tensor(stage_m[:, g, :], sgt[cb][:], qs, cacc[cb][:],
                                                                                   ALU.mult, ALU.mult),
                          reads=[t_sgt[cb], t_cacc[cb]], pw=[t_sm])
                    kb.op("dve", lambda e, g=g: e.tensor_copy(convbuf[:, g, 0:3], convbuf[:, g, 512:515]), rw=[t_conv[g]])
                for g in range(8, 16):
                    if g < 8:
                        c0 = g * 128
                    elif g < 12:
                        c0 = 2056 + (g - 8) * 128
                    else:
                        c0 = 2568 + (g - 12) * 128
                    pi = next_pm()
                    for kc in range(8):
                        kb.op("pe", lambda e, kc=kc, c0=c0, pi=pi: e.matmul(pm[pi][:, :], w_in_sb[:, kc, c0:c0 + 128], hc[:, kc, :],
                                                                          start=(kc == 0), stop=(kc == 7)),
                              reads=[t_w, t_hnT[cur]], writes=[t_pm[pi]])
                    if g < 8:
                        kb.op("act", lambda e, g=g, pi=pi: e.activation(convbuf[:, g, 3:515], pm[pi][:, :], AF.Identity),
                              reads=[t_pm[pi]], writes=[t_conv[g]])
                    elif g < 12:
                        kb.op("act", lambda e, g=g, pi=pi: e.activation(stage_a[:, g - 8, :], pm[pi][:, :], AF.Identity, scale=0.125),
                              reads=[t_pm[pi]], pw=[t_sa])
                    else:
                        kb.op("act", lambda e, g=g, pi=pi: e.activation(stage_a[:, g - 8, :], pm[pi][:, :], AF.Identity),
                              reads=[t_pm[pi]], pw=[t_sa])
                for gi, (c0, bs) in enumerate(((2048, bi_s), (2052, bf_s)) if 'gates' not in os.environ.get('KSKIP', '') else ()):
                    for kc in range(8):
                        kb.op("pe", lambda e, kc=kc, c0=c0, gi=gi: e.matmul(pg[gi][:, :], w_in_sb[:, kc, c0:c0 + 4], hc[:, kc, :],
                                                                          start=(kc == 0), stop=(kc == 7)),
                              reads=[t_w, t_hnT[cur]], writes=[t_pg[gi]])
                    kb.op("act", lambda e, gi=gi, bs=bs: e.activation(gst[:, gi, :], pg[gi][:, :], AF.Identity, bias=bs[:, 0:1]),
                          reads=[t_pg[gi], t_wc], pw=[t_gst])
                for s in range(4 if 'tm' not in os.environ.get('KSKIP', '') else 0):
                    for cg, c0 in enumerate((1024, 1536, 3080)):
                        pi = next_pm()
                        for kc in range(8):
                            kb.op("pe", lambda e, kc=kc, c0=c0, pi=pi, s=s: e.matmul(pm[pi][:, :], hc[:, kc, s * 128:(s + 1) * 128],
                                                                                   w_in_sb[:, kc, c0:c0 + 512],
                                                                                   start=(kc == 0), stop=(kc == 7)),
                                  reads=[t_w, t_hnT[cur]], writes=[t_pm[pi]])
                        if cg == 1:
                            kb.op("act", lambda e, s=s, pi=pi: e.activation(stage_t[:, s, 512:1024], pm[pi][:, :], AF.Sigmoid),
                                  reads=[t_pm[pi]], pw=[t_stt])
                        elif cg == 0:
                            kb.op("act", lambda e, s=s, pi=pi: e.activation(stage_t[:, s, 0:512], pm[pi][:, :], AF.Identity),
                                  reads=[t_pm[pi]], pw=[t_stt])
                        else:
                            kb.op("act", lambda e, s=s, pi=pi: e.activation(stage_t[:, s, 1024:1536], pm[pi][:, :], AF.Identity),
                                  reads=[t_pm[pi]], pw=[t_stt])
                kb.dma("sp", FMv[:, 0:8, i * 512:(i + 1) * 512], stage_m[:], t_FM, src=t_sm, store=True)
                kb.dma("sp", FMv[:, 8:16, i * 512:(i + 1) * 512], stage_a[:], t_FM, src=t_sa, store=True)
                kb.dma("sp", TMv[i], stage_t[:], t_TM, src=t_stt, store=True)
                kb.dma("sp", G[0:4, i * 512:(i + 1) * 512], gst[:, 0, :], t_G, src=t_gst, store=True)
                kb.dma("sp", G[4:8, i * 512:(i + 1) * 512], gst[:, 1, :], t_G, src=t_gst, store=True)
            kb.barrier()
        ph0A.close()

        if stop <= 1:
            return nc
        with ExitStack() as ph:
            gi_c = sbuf(ph, "gi_c", [NCH, 4, 64], F32)
            gf_c = sbuf(ph, "gf_c", [NCH, 4, 64], F32)
            tmpg = sbuf(ph, "tmpg", [NCH, 4, 64], F32)
            bneg = sbuf(ph, "bneg", [NCH, 4, 64], F32)
            a_t = sbuf(ph, "a_t", [NCH, 4, 64], F32)
            u_t = sbuf(ph, "u_t", [NCH, 4, 64], F32)
            fl_t = sbuf(ph, "fl_t", [NCH, 4, 64], F32)
            amax = sbuf(ph, "amax", [NCH, 4], F32)
            blast = sbuf(ph, "blast", [NCH, 4], F32)
            negM = sbuf(ph, "negM", [NCH, 4], F32)
            amaxT = sbuf(ph, "amaxT", [4, NCH], F32)
            blastT = sbuf(ph, "blastT", [4, NCH], F32)
            mrow = sbuf(ph, "mrow", [4, NCH], F32)
            Mend = sbuf(ph, "Mend", [4, NCH], F32)
            mprev = sbuf(ph, "mprev", [4, NCH], F32)
            wold = sbuf(ph, "wold", [4, NCH], F32)
            uT = sbuf(ph, "uT", [64, 4, NCH], F32)
            flT = sbuf(ph, "flT", [64, 4, NCH], F32)
            wb = sbuf(ph, "wb", [128, 4, NCH], F32)
            sel4s = sbuf(ph, "sel4s", [4, 4, 128], F32)
            gmB = sbuf(ph, "gmB", [64, 512], F32)
            mask64 = sbuf(ph, "mask64", [64, 64], F32)
            qk = [sbuf(ph, "qk%d" % i, [128, 8, 512], BF16) for i in range(2)]
            vt = [sbuf(ph, "vt%d" % i, [64, 8, 4, 129], BF16) for i in range(2)]
            gso = [sbuf(ph, "gso%d" % i, [64, 8, 512], BF16) for i in range(2)]
            accS = [sbuf(ph, "accS%d" % i, [64, 4, 129], F32) for i in range(2)]
            sa_2 = [sbuf(ph, "sa%d" % i, [64, 4, 1], F32) for i in range(2)]
            tq_2 = [sbuf(ph, "tq%d" % i, [64, 4, 1], F32) for i in range(2)]
            junkS = sbuf(ph, "junkS", [64, 128], BF16)
            so = [sbuf(ph, "so%d" % i, [64, 8, 512], BF16) for i in range(2)]
            C32 = [sbuf(ph, "C32_%d" % i, [128, 4, 129], F32) for i in range(2)]
            Csb = sbuf(ph, "Csb", [128, 4, 129], BF16)
            ktok_2 = [sbuf(ph, "ktok_%d" % i, [64, 4, 128], BF16) for i in range(2)]
            vp_2 = [sbuf(ph, "vp_%d" % i, [64, 4, 130], BF16) for i in range(2)]
            scT_2 = [sbuf(ph, "scT_%d" % i, [64, 4, 64], BF16) for i in range(2)]
            den_2 = [sbuf(ph, "den_%d" % i, [64, 4, 1], F32) for i in range(2)]
            hb_2 = [sbuf(ph, "hb_%d" % i, [64, 4, 128], F32) for i in range(2)]
            hsq_2 = [sbuf(ph, "hsq_%d" % i, [64, 4, 128], F32) for i in range(2)]
            ssq2_2 = [sbuf(ph, "ssq2_%d" % i, [64, 4, 1], F32) for i in range(2)]
            hb2_2 = [sbuf(ph, "hb2_%d" % i, [64, 4, 128], F32) for i in range(2)]
            ostage = [sbuf(ph, "ostage%d" % i, [64, 8, 512], BF16) for i in range(2)]
            psA = psum(ph, "psA", [128, 512], F32)
            pk = psum(ph, "pk", [64, 1024], BF16)
            pp = psum(ph, "pp", [64, 512], F32)
            pacc = psum(ph, "pacc", [64, 1024], F32)
            pc = psum(ph, "pc", [128, 1024], F32)
            paccv = pacc[:].rearrange("p (h e) -> p h e", h=4)
            pcv = pc[:].rearrange("p (h e) -> p h e", h=4)
            pkv = pk[:, 0:512].rearrange("p (h e) -> p h e", h=4)
            ppv = pp[:, 0:256].rearrange("p (h e) -> p h e", h=4)
            tg = T()
            t_psA = T()
            kb.dma("sp", gi_c[:], G[0:4, :].rearrange("h (c l) -> c h l", l=64), tg, src=t_G)
            kb.dma("sp", gf_c[:], G[4:8, :].rearrange("h (c l) -> c h l", l=64), tg, src=t_G)
            kb.dma("sp", sel4s[:], sel4_d[:], tg)
            kb.dma("sp", gmB[:], gml.to_broadcast([64, 512]), tg)
            kb.dma("sp", mask64[:], tri_d[0:64, 0:64], tg)
            tp = T()
            kb.op("act", lambda e: e.activation(tmpg[:], gf_c[:], AF.Exp, scale=-1.0), reads=[tg], writes=[tp])
            kb.op("act", lambda e: e.activation(tmpg[:], tmpg[:], AF.Ln, bias=1.0), rw=[tp])
            for h in range(4):
                kb.op("dve", lambda e, h=h: e.tensor_tensor_scan(bneg[:, h, :], ones_f[0:NCH, 0:64], tmpg[:, h, :], 0.0, ALU.mult, ALU.add),
                      reads=[t_ones], rw=[tp])
            kb.op("dve", lambda e: e.tensor_tensor(a_t[:], gi_c[:], bneg[:], ALU.add), reads=[tg], rw=[tp])
            kb.op("dve", lambda e: e.tensor_reduce(amax[:], a_t[:], AX.X, ALU.max), rw=[tp])
            kb.op("dve", lambda e: e.tensor_scalar(blast[:], bneg[:, :, 63], -1.0, None, ALU.mult), rw=[tp])
            kb.op("pe", lambda e: e.transpose(psA[0:4, 0:NCH], amax[:], ident_f[0:NCH, 0:NCH]), reads=[tp, t_const], writes=[t_psA])
            kb.op("dve", lambda e: e.tensor_copy(amaxT[:], psA[0:4, 0:NCH]), reads=[t_psA], rw=[tp])
            kb.op("pe", lambda e: e.transpose(psA[0:4, 0:NCH], blast[:], ident_f[0:NCH, 0:NCH]), reads=[tp, t_const], writes=[t_psA])
            kb.op("dve", lambda e: e.tensor_copy(blastT[:], psA[0:4, 0:NCH]), reads=[t_psA], rw=[tp])
            kb.op("dve", lambda e: e.tensor_tensor_scan(mrow[:], amaxT[:], blastT[:], 0.0, ALU.max, ALU.add), rw=[tp])
            kb.op("dve", lambda e: e.tensor_tensor(Mend[:], mrow[:], blastT[:], ALU.subtract), rw=[tp])
            kb.op("dve", lambda e: e.memset(mprev[:, 0:1], 0.0), rw=[tp])
            if NCH > 1:
                kb.op("dve", lambda e: e.tensor_copy(mprev[:, 1:NCH], mrow[:, 0:NCH - 1]), rw=[tp])
            kb.op("dve", lambda e: e.tensor_tensor(wold[:], mprev[:], Mend[:], ALU.subtract), rw=[tp])
            kb.op("act", lambda e: e.activation(wold[:], wold[:], AF.Exp), rw=[tp])
            kb.op("pe", lambda e: e.transpose(psA[0:NCH, 0:4], Mend[:], ident_f[0:4, 0:4]), reads=[tp, t_const], writes=[t_psA])
            kb.op("dve", lambda e: e.tensor_scalar(negM[:], psA[0:NCH, 0:4], -1.0, None, ALU.mult), reads=[t_psA], rw=[tp])
            for h in range(4):
                kb.op("act", lambda e, h=h: e.activation(u_t[:, h, :], a_t[:, h, :], AF.Exp, bias=negM[:, h:h + 1]), rw=[tp])
                kb.op("act", lambda e, h=h: e.activation(fl_t[:, h, :], bneg[:, h, :], AF.Exp, bias=negM[:, h:h + 1]), rw=[tp])
            for (src_t, dst_t) in ((u_t, uT), (fl_t, flT)):
                for h in range(4):
                    kb.op("pe", lambda e, h=h, src_t=src_t: e.transpose(psA[0:64, 0:NCH], src_t[:, h, :], ident_f[0:NCH, 0:NCH]),
                          reads=[tp, t_const], writes=[t_psA])
                    kb.op("dve", lambda e, h=h, dst_t=dst_t: e.tensor_copy(dst_t[:, h, :], psA[0:64, 0:NCH]), reads=[t_psA], rw=[tp])
            for h in range(4):
                kb.op("pe", lambda e, h=h: e.matmul(psA[:, 0:NCH], sel4s[:, h, :], wold[:], start=True, stop=True),
                      reads=[tp, tg], writes=[t_psA])
                kb.op("dve", lambda e, h=h: e.tensor_copy(wb[:, h, :], psA[:, 0:NCH]), reads=[t_psA], rw=[tp])
            t_qk, t_vt, t_so = [T(), T()], [T(), T()], [T(), T()]
            t_C32 = [T(), T()]
            t_Csb, t_pk, t_pp, t_pacc, t_pc = T(), T(), T(), T(), T()
            t_ktok2, t_vp2, t_scT2 = [T(), T()], [T(), T()], [T(), T()]
            t_den2, t_hb_2, t_hsq2, t_ssq22, t_hb22 = [T(), T()], [T(), T()], [T(), T()], [T(), T()], [T(), T()]
            t_os = [T(), T()]
            t_gso, t_accS, t_sa, t_tq, t_junkS = [T(), T()], [T(), T()], [T(), T()], [T(), T()], T()
            for i_ in range(2):
                kb.op("pool", lambda e, i_=i_: e.memset(vt[i_][:, :, :, 128:129], 1.0), writes=[t_vt[i_]])
            kb.op("dve", lambda e: e.memset(C32[1][:], 0.0), writes=[t_C32[1]])
            FMv2 = FM.rearrange("g p t -> p g t")
            TMc = TM.rearrange("(n cc l) c -> n l cc c", cc=8, l=64)
            HCc = HC.rearrange("(n cc l) c -> n l cc c", cc=8, l=64)

            def loadB(blk):
                bf = blk % 2
                kb.dma("sp", qk[bf][:], FMv2[:, 0:8, blk * 512:(blk + 1) * 512], t_qk[bf], src=t_FM)
                for h_ in range(4):
                    kb.dma("sp", vt[bf][:, :, h_, 0:128], TMc[blk][:, :, h_ * 128:(h_ + 1) * 128], t_vt[bf], src=t_TM)
                kb.dma("sp", so[bf][:], TMc[blk][:, :, 512:1024], t_so[bf], src=t_TM)
                kb.op("pool", lambda e, bf=bf: e.tensor_tensor(gso[bf][:], so[bf][:], gmB[:].unsqueeze(1).to_broadcast([64, 8, 512]), ALU.mult),
                      reads=[t_so[bf], tg], writes=[t_gso[bf]])

            def outchain(c):
                blk_, cc_ = divmod(c, 8)
                bf_, cur_ = blk_ % 2, c % 2
                aS, den, tq, hb2, sa = accS[cur_], den_2[cur_], tq_2[cur_], hb2_2[cur_], sa_2[cur_]
                t_aS, t_dn, t_tq_, t_h2, t_sa_ = t_accS[cur_], t_den2[cur_], t_tq[cur_], t_hb22[cur_], t_sa[cur_]
                for h in range(4):
                    kb.op("act", lambda e, h=h, aS=aS, sa=sa: e.activation(junkS[:], aS[:, h, 0:128], AF.Square, accum_out=sa[:, h, :]),
                          reads=[t_aS], writes=[t_junkS], pw=[t_sa_])
                kb.op("dve", lambda e, aS=aS, den=den, c=c: e.tensor_tensor(den[:], aS[:, :, 128:129], flT[:, :, c:c + 1], ALU.max),
                      reads=[t_aS, tp], writes=[t_dn])
                kb.op("dve", lambda e, aS=aS, den=den: e.scalar_tensor_tensor(den[:], aS[:, :, 128:129], -1.0, den[:], ALU.mult, ALU.max),
                      reads=[t_aS], rw=[t_dn])
                kb.op("dve", lambda e, den=den: e.reciprocal(den[:], den[:]), rw=[t_dn])
                kb.op("dve", lambda e, tq=tq, sa=sa, den=den: e.tensor_tensor(tq[:], sa[:], den[:], ALU.mult), reads=[t_sa_, t_dn], writes=[t_tq_])
                kb.op("dve", lambda e, tq=tq, den=den: e.tensor_tensor(tq[:], tq[:], den[:], ALU.mult), reads=[t_dn], rw=[t_tq_])
                kb.op("act", lambda e, tq=tq: e.activation(tq[:], tq[:], AF.Sqrt, scale=1.0 / 128, bias=EPS), rw=[t_tq_])
                kb.op("dve", lambda e, tq=tq: e.reciprocal(tq[:], tq[:]), rw=[t_tq_])
                kb.op("dve", lambda e, tq=tq, den=den: e.tensor_tensor(tq[:], tq[:], den[:], ALU.mult), reads=[t_dn], rw=[t_tq_])
                kb.op("dve", lambda e, hb2=hb2, aS=aS, tq=tq: e.tensor_tensor(hb2[:], aS[:, :, 0:128], tq[:].to_broadcast([64, 4, 128]), ALU.mult),
                      reads=[t_aS, t_tq_], writes=[t_h2])
                kb.op("pool", lambda e, hb2=hb2, bf_=bf_, cc_=cc_: e.tensor_tensor(ostage[bf_][:, cc_, :].rearrange("p (h e) -> p h e", h=4), hb2[:],
                                                                              gso[bf_][:, cc_, :].rearrange("p (h e) -> p h e", h=4), ALU.mult),
                      reads=[t_h2, t_gso[bf_]], pw=[t_os[bf_]])
                if cc_ == 7:
                    kb.dma("sp", HCc[blk_][:, :, 0:512], ostage[bf_][:], t_HC, src=t_os[bf_], store=True)

            loadB(0)
            for blk in range(NT):
                bf = blk % 2
                for cc in range(8):
                    c = blk * 8 + cc
                    cur, prv = c % 2, 1 - (c % 2)
                    ktok, vp, scT, den, hb, hsq, ssq2, hb2 = ktok_2[cur], vp_2[cur], scT_2[cur], den_2[cur], hb_2[cur], hsq_2[cur], ssq2_2[cur], hb2_2[cur]
                    t_ktok, t_vp, t_scT, t_den, t_hb, t_hsq, t_ssq2, t_hb2 = t_ktok2[cur], t_vp2[cur], t_scT2[cur], t_den2[cur], t_hb_2[cur], t_hsq2[cur], t_ssq22[cur], t_hb22[cur]
                    sl = slice(cc * 64, (cc + 1) * 64)
                    for h in range(4):
                        kb.op("act", lambda e, ktok=ktok, vp=vp, scT=scT, den=den, hb=hb, hsq=hsq, ssq2=ssq2, hb2=hb2, h=h, prv=prv, c=c: e.activation(Csb[:, h, :], C32[prv][:, h, :], AF.Identity,
                                                                              scale=wb[:, h, c:c + 1]),
                              reads=[t_C32[prv], tp], pw=[t_Csb])
                    for h in range(4):
                        kb.op("pe", lambda e, ktok=ktok, vp=vp, scT=scT, den=den, hb=hb, hsq=hsq, ssq2=ssq2, hb2=hb2, h=h, bf=bf, sl=sl: e.transpose(pkv[:, h, :], qk[bf][:, 4 + h, sl], ident_b[:]),
                              reads=[t_qk[bf], t_const], writes=[t_pk])
                    kb.op("act", lambda e, ktok=ktok, vp=vp, scT=scT, den=den, hb=hb, hsq=hsq, ssq2=ssq2, hb2=hb2: e.activation(ktok[:], pkv, AF.Identity), reads=[t_pk], writes=[t_ktok])
                    kb.op("dve", lambda e, vp=vp, bf=bf, cc=cc, c=c: e.tensor_tensor(vp[:, :, 0:129], vt[bf][:, cc, :, :],
                                                                          uT[:, :, c:c + 1].to_broadcast([64, 4, 129]), ALU.mult),
                          reads=[t_vt[bf], tp], writes=[t_vp])
                    for h in range(4):
                        kb.op("pe", lambda e, ktok=ktok, vp=vp, scT=scT, den=den, hb=hb, hsq=hsq, ssq2=ssq2, hb2=hb2, h=h, bf=bf, sl=sl: e.matmul(ppv[:, h, :], qk[bf][:, 4 + h, sl], qk[bf][:, h, sl], start=True, stop=True),
                              reads=[t_qk[bf]], writes=[t_pp])
                    kb.op("dve", lambda e, ktok=ktok, vp=vp, scT=scT, den=den, hb=hb, hsq=hsq, ssq2=ssq2, hb2=hb2: e.tensor_tensor(scT[:], ppv, mask64[:].unsqueeze(1).to_broadcast([64, 4, 64]), ALU.mult),
                          reads=[t_pp, tg], writes=[t_scT])
                    for h in range(4):
                        kb.op("pe", lambda e, ktok=ktok, vp=vp, scT=scT, den=den, hb=hb, hsq=hsq, ssq2=ssq2, hb2=hb2, h=h, bf=bf, sl=sl: e.matmul(paccv[:, h, 0:129], qk[bf][:, h, sl], Csb[:, h, :], start=True, stop=False),
                              reads=[t_qk[bf], t_Csb], writes=[t_pacc])
                        kb.op("pe", lambda e, ktok=ktok, vp=vp, scT=scT, den=den, hb=hb, hsq=hsq, ssq2=ssq2, hb2=hb2, h=h: e.matmul(paccv[:, h, 0:129], scT[:, h, :], vp[:, h, 0:129], start=False, stop=True),
                              reads=[t_scT, t_vp], writes=[t_pacc])
                    for h in range(4):
                        kb.op("pe", lambda e, ktok=ktok, vp=vp, scT=scT, den=den, hb=hb, hsq=hsq, ssq2=ssq2, hb2=hb2, h=h: e.matmul(pcv[:, h, 0:129], ktok[:, h, :], vp[:, h, 0:129], start=True, stop=True),
                              reads=[t_ktok, t_vp], writes=[t_pc])
                    for h in range(4):
                        kb.op("dve", lambda e, ktok=ktok, vp=vp, scT=scT, den=den, hb=hb, hsq=hsq, ssq2=ssq2, hb2=hb2, h=h, cur=cur, prv=prv, c=c: e.scalar_tensor_tensor(C32[cur][:, h, :], C32[prv][:, h, :], wb[:, h, c:c + 1],
                                                                                               pcv[:, h, 0:129], ALU.mult, ALU.add),
                              reads=[t_C32[prv], t_pc, tp], pw=[t_C32[cur]])
                    kb.op("act", lambda e, cur=cur: e.activation(accS[cur][:], paccv[:, :, 0:129], AF.Identity), reads=[t_pacc], writes=[t_accS[cur]])
                    if c > 0:
                        outchain(c - 1)
                    if cc == 0 and blk + 1 < NT:
                        loadB(blk + 1)
            outchain(NCH - 1)
            kb.barrier()

        if stop <= 2:
            return nc
        with ExitStack() as ph:
            KT = [sbuf(ph, "KT%d" % i, [128, S], BF16) for i in range(2)]
            VA = [sbuf(ph, "VA%d" % i, [128, NS, 65], BF16) for i in range(2)]
            QT = [sbuf(ph, "QT%d" % i, [128, 512], BF16) for i in range(2)]
            kms = sbuf(ph, "kms", [64, NB], F32)
            kmh = sbuf(ph, "kmh", [128, NB], BF16)
            kml = sbuf(ph, "kml", [128, NB], BF16)
            kmd = sbuf(ph, "kmd", [64, NB], F32)
            gsb = [sbuf(ph, "gsb%d" % i, [128, max(NB, 8)], F32) for i in range(4)]
            m8_4 = sbuf(ph, "m8_4", [128, 4, 8], F32)
            gsb4 = sbuf(ph, "gsb4", [128, 4, max(NB, 8)], F32)
            t_gsb4 = T()
            selq = [sbuf(ph, "selq%d" % i, [128, 4, max(NB, 8)], F32) for i in range(2)]
            tmpA = [sbuf(ph, "tmpA%d" % i, [128, 4, 65], F32) for i in range(2)]
            PT = [sbuf(ph, "PT%d" % i, [128, 512], BF16) for i in range(4)]
            accA = [sbuf(ph, "accA%d" % i, [128, 4, 65], F32) for i in range(2)]
            rdn = sbuf(ph, "rdn", [128, 4, 1], F32)
            oa = [sbuf(ph, "oa%d" % i, [128, 4, 64], BF16) for i in range(2)]
            pS = [psum(ph, "pS%d" % i, [128, 512], F32) for i in range(4)]
            pO = [psum(ph, "pO%d" % i, [128, 512], F32) for i in range(2)]
            pG = psum(ph, "pG", [128, 512], F32)
            t_KT, t_VA, t_QT = [T(), T()], [T(), T()], [T(), T()]
            t_km, t_gsb, t_m8, t_sel = T(), [T() for _ in range(4)], T(), [T(), T()]
            t_tmpA = [T(), T()]
            cnt_t = [0]
            t_PT, t_pS, t_pO = [T() for _ in range(4)], [T() for _ in range(4)], [T(), T()]
            t_pG, t_acc, t_rdn, t_oa = T(), [T(), T()], T(), [T(), T()]
            FMp = FM
            TMn = TM.rearrange("(n p) c -> p n c", p=128)
            HCq = HC.rearrange("(n s p) c -> n p s c", s=4, p=128)
            for i in range(2):
                kb.op("pool", lambda e, i=i: e.memset(VA[i][:, :, 64:65], 1.0), writes=[t_VA[i]])
                kb.op("pool", lambda e, i=i: e.memset(KT[i][64:128, :], 0.0), writes=[t_KT[i]])
                kb.op("pool", lambda e, i=i: e.memset(QT[i][64:128, :], 0.0), writes=[t_QT[i]])
            kb.op("pool", lambda e: e.memset(kmh[:], 0.0), writes=[t_km])
            kb.op("pool", lambda e: e.memset(kml[:], 0.0), rw=[t_km])

            def loadH(h):
                bf = h % 2
                kb.dma("sp", KT[bf][0:64, :], FMp[12 + h // 2, (h % 2) * 64:(h % 2) * 64 + 64, :], t_KT[bf], src=t_FM)
                kb.dma("sp", VA[bf][:, :, 0:64], TMn[:, :, 1024 + h * 64:1024 + (h + 1) * 64], t_VA[bf], src=t_TM)

            cnt_s, cnt_o, cnt_p, cnt_q = [0], [0], [0], [0]
            loadH(0)
            for h in range(8):
                hb_ = h % 2
                if h + 1 < 8:
                    loadH(h + 1)
                Kc, Vc = KT[hb_], VA[hb_]
                kb.op("dve", lambda e, Kc=Kc: e.tensor_reduce(kms[:], Kc[0:64, :].rearrange("p (n k) -> p n k", k=256), AX.X, ALU.add),
                      reads=[t_KT[hb_]], writes=[t_km])
                kb.op("dve", lambda e: e.tensor_copy(kmh[0:64, :], kms[:]), rw=[t_km])
                kb.op("dve", lambda e: e.tensor_tensor(kmd[:], kms[:], kmh[0:64, :], ALU.subtract), rw=[t_km])
                kb.op("dve", lambda e: e.tensor_copy(kml[0:64, :], kmd[:]), rw=[t_km])
                units = []
                for i in range(NT):
                    for qs in range(4):
                        for kh in ([0] if qs % 2 == 0 else [0, 1]):
                            units.append(("own", i, qs, kh))
                    for b in range(2 * i + 1):
                        for kh in range(2):
                            units.append(("past", i, b, kh))

                def loadQ(i):
                    kb.dma("sp", QT[i % 2][0:64, :], FMp[8 + h // 2, (h % 2) * 64:(h % 2) * 64 + 64, i * 512:(i + 1) * 512], t_QT[i % 2], src=t_FM)

                def gating(i):
                    qb = i % 2
                    Qc = QT[qb]
                    sq, ts_ = selq[i % 2], t_sel[i % 2]
                    pGv = pG[:, 0:4 * 32].rearrange("p (q n) -> p q n", q=4)
                    big = [qs for qs in range(4) if 2 * i + qs // 2 > 3]
                    for qs in range(4):
                        own = 2 * i + qs // 2
                        if 0 < own <= 3:
                            kb.op("dve", lambda e, qs=qs, sq=sq: e.memset(sq[:, qs, :], 1.0), pw=[ts_])
                    if not big:
                        return
                    for qs in big:
                        kb.op("pe", lambda e, qs=qs, Qc=Qc: e.matmul(pG[:, qs * 32:qs * 32 + NB], Qc[:, qs * 128:(qs + 1) * 128], kmh[:], start=True, stop=False),
                              reads=[t_QT[qb], t_km], writes=[t_pG])
                        kb.op("pe", lambda e, qs=qs, Qc=Qc: e.matmul(pG[:, qs * 32:qs * 32 + NB], Qc[:, qs * 128:(qs + 1) * 128], kml[:], start=False, stop=True),
                              reads=[t_QT[qb], t_km], writes=[t_pG])
                    kb.op("dve", lambda e: e.memset(gsb4[:], -BIG), writes=[t_gsb4])
                    for p in range(2):
                        own = 2 * i + p
                        if own > 3:
                            kb.op("dve", lambda e, p=p, own=own: e.tensor_copy(gsb4[:, 2 * p:2 * p + 2, 0:own], pGv[:, 2 * p:2 * p + 2, 0:own]),
                                  reads=[t_pG], pw=[t_gsb4])
                    for qs in big:
                        kb.op("dve", lambda e, qs=qs: e.max(m8_4[:, qs, :], gsb4[:, qs, :]), reads=[t_gsb4], pw=[t_m8])
                    for p in range(2):
                        own = 2 * i + p
                        if own > 3:
                            kb.op("dve", lambda e, p=p, own=own, sq=sq: e.tensor_tensor(sq[:, 2 * p:2 * p + 2, 0:own], gsb4[:, 2 * p:2 * p + 2, 0:own],
                                                                                     m8_4[:, 2 * p:2 * p + 2, 2:3].to_broadcast([128, 2, own]), ALU.is_ge),
                                  reads=[t_gsb4, t_m8], pw=[ts_])

                def stageA(u):
                    kind, i, x_, kh = u
                    qb = i % 2
                    Qc = QT[qb]
                    sb_ = cnt_s[0] % 4
                    cnt_s[0] += 1
                    if kind == "own":
                        qs = x_
                        own = 2 * i + qs // 2
                        k0 = own * 256 + kh * 128
                        kb.op("pe", lambda e, sb_=sb_, k0=k0, qs=qs, Qc=Qc: e.matmul(pS[sb_][:, 0:128], Kc[:, k0:k0 + 128], Qc[:, qs * 128:(qs + 1) * 128],
                                                                                  start=True, stop=True),
                              reads=[t_KT[hb_], t_QT[qb]], writes=[t_pS[sb_]])
                        kb.op("act", lambda e, sb_=sb_: e.activation(PT[sb_][:, 0:128], pS[sb_][:, 0:128], AF.Exp),
                              reads=[t_pS[sb_]], writes=[t_PT[sb_]])
                        if kh == qs % 2:
                            kb.op("pool", lambda e, sb_=sb_: e.tensor_tensor(PT[sb_][:, 0:128], PT[sb_][:, 0:128], tri_b[:], ALU.mult),
                                  reads=[t_const], rw=[t_PT[sb_]])
                    else:
                        b = x_
                        q0 = 0 if b < 2 * i else 2
                        k0 = b * 256 + kh * 128
                        kb.op("pe", lambda e, sb_=sb_, k0=k0, q0=q0, Qc=Qc: e.matmul(pS[sb_][:, q0 * 128:512], Kc[:, k0:k0 + 128], Qc[:, q0 * 128:512],
                                                                                  start=True, stop=True),
                              reads=[t_KT[hb_], t_QT[qb]], writes=[t_pS[sb_]])
                        kb.op("act", lambda e, sb_=sb_, q0=q0: e.activation(PT[sb_][:, q0 * 128:512], pS[sb_][:, q0 * 128:512], AF.Exp),
                              reads=[t_pS[sb_]], writes=[t_PT[sb_]])
                    return sb_

                def stageB(u, sb_):
                    kind, i, x_, kh = u
                    ac, t_ac = accA[i % 2], t_acc[i % 2]
                    if kh == 0:
                        cnt_o[0] += 1
                    ob = cnt_o[0] % 2
                    if kind == "own":
                        qs = x_
                        own = 2 * i + qs // 2
                        last = 0 if qs % 2 == 0 else 1
                        kb.op("pe", lambda e, ob=ob, sb_=sb_, kh=kh, own=own, last=last: e.matmul(pO[ob][:, 0:65], PT[sb_][:, 0:128], Vc[:, own * 2 + kh, :],
                                                                                              start=(kh == 0), stop=(kh == last)),
                              reads=[t_PT[sb_], t_VA[hb_]], writes=[t_pO[ob]])
                        if kh == last:
                            kb.op("dve", lambda e, ob=ob, qs=qs, ac=ac: e.tensor_copy(ac[:, qs, :], pO[ob][:, 0:65]), reads=[t_pO[ob]], pw=[t_ac])
                    else:
                        b = x_
                        q0 = 0 if b < 2 * i else 2
                        for qs in range(q0, 4):
                            kb.op("pe", lambda e, ob=ob, sb_=sb_, kh=kh, qs=qs, b=b, q0=q0: e.matmul(pO[ob][:, qs * 128:qs * 128 + 65], PT[sb_][:, qs * 128:(qs + 1) * 128],
                                                                                                  Vc[:, b * 2 + kh, :], start=(kh == 0 and qs == q0), stop=(kh == 1),
                                                                                                  skip_group_check=True),
                                  reads=[t_PT[sb_], t_VA[hb_]], writes=[t_pO[ob]])
                        if kh == 1:
                            tb = cnt_t[0] % 2
                            cnt_t[0] += 1
                            pov = pO[ob][:].rearrange("p (q e) -> p q e", q=4)
                            nq = 4 - q0
                            kb.op("dve", lambda e, tb=tb, pov=pov, q0=q0, nq=nq, b=b, i=i: e.tensor_tensor(
                                tmpA[tb][:, q0:4, :], pov[:, q0:4, 0:65], selq[i % 2][:, q0:4, b:b + 1].to_broadcast([128, nq, 65]), ALU.mult),
                                reads=[t_pO[ob], t_sel[i % 2]], writes=[t_tmpA[tb]])
                            kb.op("dve", lambda e, tb=tb, q0=q0, ac=ac: e.tensor_tensor(ac[:, q0:4, :], ac[:, q0:4, :], tmpA[tb][:, q0:4, :], ALU.add),
                                  reads=[t_tmpA[tb]], rw=[t_ac])

                def finalize(i):
                    ac, t_ac = accA[i % 2], t_acc[i % 2]
                    ab = i % 2
                    kb.op("dve", lambda e, ac=ac: e.reciprocal(rdn[:], ac[:, :, 64:65]), reads=[t_ac], writes=[t_rdn])
                    kb.op("dve", lambda e, ab=ab, ac=ac: e.tensor_tensor(oa[ab][:], ac[:, :, 0:64], rdn[:].to_broadcast([128, 4, 64]), ALU.mult),
                          reads=[t_ac, t_rdn], writes=[t_oa[ab]])
                    kb.dma("sp", HCq[i][:, :, 512 + h * 64:512 + (h + 1) * 64], oa[ab][:], t_HC, src=t_oa[ab], store=True)

                def enter_tile(i):
                    if i + 1 < NT:
                        loadQ(i + 1)
                    gating(i)

                loadQ(0)
                LA = 2
                entered = set()

                def issueA(n):
                    ti_ = units[n][1]
                    if ti_ not in entered:
                        entered.add(ti_)
                        enter_tile(ti_)
                    return stageA(units[n])

                sbs = {}
                for n in range(min(LA, len(units))):
                    sbs[n] = issueA(n)
                for n, u in enumerate(units):
                    if n + LA < len(units):
                        sbs[n + LA] = issueA(n + LA)
                    stageB(u, sbs.pop(n))
                    if n + 1 == len(units) or units[n + 1][1] != u[1]:
                        finalize(u[1])
            kb.barrier()

        if stop <= 3:
            return nc
        with ExitStack() as ph:
            w_out_sb = sbuf(ph, "w_out_sb", [128, 8, D], BF16)
            w_r_sb = sbuf(ph, "w_r_sb", [128, 8, 36], BF16)
            brB = sbuf(ph, "brB", [128, 36], F32)
            hct = [sbuf(ph, "hct%d" % i, [128, 4, D], BF16) for i in range(2)]
            xt = [sbuf(ph, "xtD%d" % i, [128, 4, D], F32) for i in range(2)]
            hcT = sbuf(ph, "hcT", [128, 8, 512], BF16)
            x1 = sbuf(ph, "x1", [128, 4, D], F32)
            tmpD2 = [sbuf(ph, "tmpD%d" % i, [128, 512], F32) for i in range(2)]
            junk = sbuf(ph, "junkD", [128, D], BF16)
            ssq = sbuf(ph, "ssqD", [128, 4], F32)
            rs = sbuf(ph, "rsD", [128, 4], F32)
            xn = sbuf(ph, "xnD", [128, 4, D], BF16)
            hn2T = [sbuf(ph, "hn2T%d" % i, [128, 8, 512], BF16) for i in range(2)]
            lg = sbuf(ph, "lg", [128, 36], F32)
            lmax = sbuf(ph, "lmax", [128, 1], F32)
            nlmax = sbuf(ph, "nlmax", [128, 1], F32)
            eg = sbuf(ph, "eg", [128, 4], F32)
            gsum = sbuf(ph, "gsum", [128, 1], F32)
            ohg = sbuf(ph, "ohg", [128, 4], F32)
            me = sbuf(ph, "me", [128, 32], F32)
            m8r = sbuf(ph, "m8r", [128, 8], F32)
            e2 = sbuf(ph, "e2", [128, 1], F32)
            w1 = sbuf(ph, "w1", [128, 1], F32)
            w2 = sbuf(ph, "w2", [128, 1], F32)
            t1 = sbuf(ph, "t1", [128, 32], F32)
            t2 = sbuf(ph, "t2", [128, 32], F32)
            pT = [psum(ph, "pTD%d" % i, [128, 1024], BF16) for i in range(2)]
            pm = [psum(ph, "pmD%d" % i, [128, 512], F32) for i in range(4)]
            pr = psum(ph, "prD", [128, 512], F32)
            t_w, t_hct, t_xt = T(), [T(), T()], [T(), T()]
            t_hcT, t_x1, t_tmp2, t_junk, t_ssq, t_rs, t_xn = T(), T(), [T(), T()], T(), T(), T(), T()
            t_hn2T, t_pT, t_pm, t_pr, t_r = [T(), T()], [T(), T()], [T() for _ in range(4)], T(), T()
            kb.dma("pool", w_out_sb[:], w_out.rearrange("(kc p) n -> p kc n", p=128), t_w)
            kb.dma("pool", w_r_sb[:], w_r.rearrange("(kc p) n -> p kc n", p=128), t_w)
            kb.dma("sp", brB[:], b_r.to_broadcast([128, 36]), t_w)
            xv = x.rearrange("(n s p) d -> n p s d", s=4, p=128)
            HCv = HC.rearrange("(n s p) d -> n p s d", s=4, p=128)
            X1v = X1.rearrange("(n s p) d -> n p s d", s=4, p=128)
            XNv = XN.rearrange("(n s p) d -> n p s d", s=4, p=128)
            kb.dma("sp", hct[0][:], HCv[0], t_hct[0], src=t_HC)
            kb.dma("sp", xt[0][:], xv[0], t_xt[0])
            def router(i):
                cur = i % 2
                hc2 = hn2T[cur]
                for s in range(4):
                    ti = i * 4 + s
                    for kc in range(8):
                        kb.op("pe", lambda e, kc=kc, s=s: e.matmul(pr[:, 0:36], hc2[:, kc, s * 128:(s + 1) * 128], w_r_sb[:, kc, :], start=(kc == 0), stop=(kc == 7)),
                              reads=[t_hn2T[cur], t_w], writes=[t_pr])
                    kb.op("dve", lambda e: e.tensor_tensor(lg[:], pr[:, 0:36], brB[:], ALU.add), reads=[t_pr, t_w], writes=[t_r])
                    kb.op("dve", lambda e: e.tensor_reduce(lmax[:], lg[:, 0:4], AX.X, ALU.max), rw=[t_r])
                    kb.op("dve", lambda e: e.tensor_scalar(nlmax[:], lmax[:], -1.0, None, ALU.mult), rw=[t_r])
                    kb.op("act", lambda e: e.activation(eg[:], lg[:, 0:4], AF.Exp, bias=nlmax[:, 0:1], accum_out=gsum[:]), rw=[t_r])
                    kb.op("dve", lambda e: e.reciprocal(gsum[:], gsum[:]), rw=[t_r])
                    kb.op("dve", lambda e: e.tensor_scalar(ohg[:], lg[:, 0:4], lmax[:, 0:1], None, ALU.is_ge), rw=[t_r])
                    kb.op("dve", lambda e: e.tensor_scalar(ohg[:], ohg[:], BIG, -BIG, ALU.mult, ALU.add), rw=[t_r])
                    kb.op("dve", lambda e: e.tensor_tensor(me[:].rearrange("p (g k) -> p g k", g=4), lg[:, 4:36].rearrange("p (g k) -> p g k", g=4),
                                                           ohg[:].unsqueeze(2).to_broadcast([128, 4, 8]), ALU.add), rw=[t_r])
                    kb.op("dve", lambda e: e.max(m8r[:], me[:]), rw=[t_r])
                    kb.op("dve", lambda e: e.tensor_tensor(e2[:], m8r[:, 1:2], m8r[:, 0:1], ALU.subtract), rw=[t_r])
                    kb.op("act", lambda e: e.activation(e2[:], e2[:], AF.Exp), rw=[t_r])
                    kb.op("dve", lambda e: e.tensor_scalar(w1[:], e2[:], 1.0, None, ALU.add), rw=[t_r])
                    kb.op("dve", lambda e: e.reciprocal(w1[:], w1[:]), rw=[t_r])
                    kb.op("dve", lambda e: e.tensor_tensor(w1[:], w1[:], gsum[:], ALU.mult), rw=[t_r])
                    kb.op("dve", lambda e: e.tensor_tensor(w2[:], w1[:], e2[:], ALU.mult), rw=[t_r])
                    kb.op("dve", lambda e, ti=ti: e.tensor_scalar(OH1a[:, ti, :], me[:], m8r[:, 0:1], None, ALU.is_equal), reads=[t_r], pw=[t_rt])
                    kb.op("dve", lambda e, ti=ti: e.tensor_scalar(OH2a[:, ti, :], me[:], m8r[:, 1:2], None, ALU.is_equal), reads=[t_r], pw=[t_rt])
                    kb.op("dve", lambda e, ti=ti: e.tensor_copy(w1a[:, ti:ti + 1], w1[:]), reads=[t_r], pw=[t_rt])
                    kb.op("dve", lambda e, ti=ti: e.tensor_copy(w2a[:, ti:ti + 1], w2[:]), reads=[t_r], pw=[t_rt])

            pmi = [0]
            for i in range(NT):
                cur = i % 2
                if i + 1 < NT:
                    kb.dma("sp", hct[1 - cur][:], HCv[i + 1], t_hct[1 - cur], src=t_HC)
                    kb.dma("sp", xt[1 - cur][:], xv[i + 1], t_xt[1 - cur])
                for kc in range(8):
                    pb = kc % 2
                    for s in range(4):
                        kb.op("pe", lambda e, kc=kc, s=s, pb=pb, cur=cur: e.transpose(pT[pb][:, s * 128:(s + 1) * 128], hct[cur][:, s, kc * 128:(kc + 1) * 128], ident_b[:]),
                              reads=[t_hct[cur], t_const], writes=[t_pT[pb]])
                    kb.op("act", lambda e, kc=kc, pb=pb: e.activation(hcT[:, kc, :], pT[pb][:, 0:512], AF.Identity), reads=[t_pT[pb]], pw=[t_hcT])
                for s in range(4):
                    for hf in range(2):
                        pi = pmi[0] % 4
                        pmi[0] += 1
                        for kc in range(8):
                            kb.op("pe", lambda e, kc=kc, s=s, hf=hf, pi=pi: e.matmul(pm[pi][:, :], hcT[:, kc, s * 128:(s + 1) * 128], w_out_sb[:, kc, hf * 512:(hf + 1) * 512],
                                                                                   start=(kc == 0), stop=(kc == 7)),
                                  reads=[t_hcT, t_w], writes=[t_pm[pi]])
                        tmpD, t_tmp = tmpD2[pi % 2], t_tmp2[pi % 2]
                        kb.op("dve", lambda e, hf=hf, pi=pi, tmpD=tmpD: e.tensor_tensor(tmpD[:], pm[pi][:, :], gate1B[:, hf * 512:(hf + 1) * 512], ALU.mult),
                              reads=[t_pm[pi], t_mod], writes=[t_tmp])
                        kb.op("pool", lambda e, s=s, hf=hf, cur=cur, tmpD=tmpD: e.tensor_tensor(x1[:, s, hf * 512:(hf + 1) * 512], tmpD[:], xt[cur][:, s, hf * 512:(hf + 1) * 512], ALU.add),
                              reads=[t_tmp, t_xt[cur]], pw=[t_x1])
                kb.dma("sp", X1v[i], x1[:], t_X1, src=t_x1, store=True)
                if i > 0:
                    router(i - 1)
                for s in range(4):
                    kb.op("act", lambda e, s=s: e.activation(junk[:], x1[:, s, :], AF.Square, accum_out=ssq[:, s:s + 1]), reads=[t_x1], writes=[t_junk, t_ssq])
                kb.op("act", lambda e: e.activation(rs[:], ssq[:], AF.Sqrt, scale=1.0 / D, bias=EPS), reads=[t_ssq], writes=[t_rs])
                kb.op("dve", lambda e: e.reciprocal(rs[:], rs[:]), rw=[t_rs])
                for s in range(4):
                    kb.op("dve", lambda e, s=s: e.tensor_scalar(xn[:, s, :], x1[:, s, :], rs[:, s:s + 1], None, ALU.mult), reads=[t_x1, t_rs], pw=[t_xn])
                hc2 = hn2T[cur]
                for kc in range(8):
                    pb = kc % 2
                    for s in range(4):
                        kb.op("pe", lambda e, kc=kc, s=s, pb=pb: e.transpose(pT[pb][:, s * 128:(s + 1) * 128], xn[:, s, kc * 128:(kc + 1) * 128], ident_b[:]),
                              reads=[t_xn, t_const], writes=[t_pT[pb]])
                    if kc % 2 == 0:
                        kb.op("act", lambda e, kc=kc, pb=pb: e.activation(hc2[:, kc, :], pT[pb][:, 0:512], AF.Identity, scale=a2[:, kc:kc + 1], bias=b2[:, kc:kc + 1]),
                              reads=[t_pT[pb], t_mod], pw=[t_hn2T[cur]])
                    else:
                        kb.op("dve", lambda e, kc=kc, pb=pb: e.tensor_scalar(hc2[:, kc, :], pT[pb][:, 0:512], a2[:, kc:kc + 1], b2[:, kc:kc + 1], ALU.mult, ALU.add),
                              reads=[t_pT[pb], t_mod], pw=[t_hn2T[cur]])
                kb.dma("sp", XNv[i], xn[:], t_XN, src=t_xn, store=True)
            router(NT - 1)
            kb.barrier()

        if stop <= 4:
            return nc
        with ExitStack() as ph:
            OHs = sbuf(ph, "OHs", [128, NS, NEXP], F32)
            OHsb = sbuf(ph, "OHsb", [128, NS, NEXP], BF16)
            Cum = sbuf(ph, "Cum", [128, NS, NEXP], F32)
            prod = sbuf(ph, "prod", [128, NS, NEXP], F32)
            R = sbuf(ph, "R", [128, NEXP], F32)
            trs_b = sbuf(ph, "trs_b", [128, 128], BF16)
            ones_b = sbuf(ph, "ones_b", [128, 128], BF16)
            thr = sbuf(ph, "thr_s", [128, NBLK], F32)
            hp = sbuf(ph, "hp_s", [128, 2], F32)
            rank1 = sbuf(ph, "rank1", [128, NS], F32)
            rank2 = sbuf(ph, "rank2", [128, NS], F32)
            base = sbuf(ph, "base", [128, NS], F32)
            ci = sbuf(ph, "ci", [128, NEXP], I32)
            padf = sbuf(ph, "padf", [128, NEXP], F32)
            pend = sbuf(ph, "pend", [128, NEXP], F32)
            pstart = sbuf(ph, "pstart", [128, NEXP], F32)
            T3 = sbuf(ph, "T3", [128, NBLK, NEXP], F32)
            be = sbuf(ph, "be", [128, NBLK], F32)
            psC = psum(ph, "psC", [128, 512], F32)
            psR = psum(ph, "psR", [128, 512], F32)
            tq, t_psC, t_psR, t_R, t_Cum = T(), T(), T(), T(), T()
            kb.dma("pool", trs_b[:], trs_d[:], tq)
            kb.dma("sp", thr[:], thr_d[:], tq)
            kb.dma("sp", hp[:], hp_d[:], tq)
            kb.op("pool", lambda e: e.memset(ones_b[:], 1.0), writes=[T()])
            t_ob = T()
            kb.op("dve", lambda e: e.memset(R[:], 0.0), writes=[t_R])
            kb.op("dve", lambda e: e.tensor_tensor(OHs[:], OH1a[:], OH2a[:], ALU.add), reads=[t_rt], writes=[t_ob])
            kb.op("dve", lambda e: e.tensor_copy(OHsb[:], OHs[:]), rw=[t_ob])
            kb.barrier()
            for ti in range(NS):
                kb.op("pe", lambda e, ti=ti: e.matmul(psC[:, 0:NEXP], trs_b[:], OHsb[:, ti, :], start=True, stop=True), reads=[t_ob, tq], writes=[t_psC])
                kb.op("pe", lambda e, ti=ti: e.matmul(psR[:, 0:NEXP], ones_b[:], OHsb[:, ti, :], start=True, stop=True), reads=[t_ob], writes=[t_psR])
                kb.op("dve", lambda e, ti=ti: e.tensor_tensor(Cum[:, ti, :], psC[:, 0:NEXP], R[:], ALU.add), reads=[t_psC, t_R], rw=[t_Cum])
                kb.op("dve", lambda e: e.tensor_tensor(R[:], R[:], psR[:, 0:NEXP], ALU.add), reads=[t_psR], rw=[t_R])
            tz = T()
            kb.op("dve", lambda e: e.tensor_tensor(prod[:], OH1a[:], Cum[:], ALU.mult), reads=[t_rt, t_Cum], writes=[tz])
            kb.op("dve", lambda e: e.tensor_reduce(rank1[:], prod[:], AX.X, ALU.add), rw=[tz])
            kb.op("dve", lambda e: e.tensor_tensor(prod[:], OH2a[:], Cum[:], ALU.mult), reads=[t_rt, t_Cum], rw=[tz])
            kb.op("dve", lambda e: e.tensor_reduce(rank2[:], prod[:], AX.X, ALU.add), rw=[tz])
            kb.op("dve", lambda e: e.tensor_scalar(ci[:], R[:], 511.0, None, ALU.add), reads=[t_R], rw=[tz])
            kb.op("dve", lambda e: e.tensor_scalar(ci[:], ci[:], 9, None, ALU.arith_shift_right), rw=[tz])
            kb.op("dve", lambda e: e.tensor_scalar(ci[:], ci[:], 9, None, ALU.logical_shift_left), rw=[tz])
            kb.op("dve", lambda e: e.tensor_copy(padf[:], ci[:]), rw=[tz])
            kb.op("dve", lambda e: e.tensor_tensor_scan(pend[:], ones_f[:, 0:NEXP], padf[:], 0.0, ALU.mult, ALU.add), reads=[t_ones], rw=[tz])
            kb.op("dve", lambda e: e.tensor_tensor(pstart[:], pend[:], padf[:], ALU.subtract), rw=[tz])
            for (OHx, rk, dsti) in ((OH1a, rank1, dest1i), (OH2a, rank2, dest2i)):
                kb.op("dve", lambda e, OHx=OHx: e.tensor_tensor(prod[:], OHx[:], pstart[:].unsqueeze(1).to_broadcast([128, NS, NEXP]), ALU.mult),
                      reads=[t_rt], rw=[tz])
                kb.op("dve", lambda e: e.tensor_reduce(base[:], prod[:], AX.X, ALU.add), rw=[tz])
                kb.op("dve", lambda e, rk=rk, dsti=dsti: e.tensor_tensor(dsti[:], base[:], rk[:], ALU.add), reads=[tz], rw=[t_rt])
            kb.op("dve", lambda e: e.tensor_tensor(T3[:], pend[:].unsqueeze(1).to_broadcast([128, NBLK, NEXP]),
                                                   thr[:].unsqueeze(2).to_broadcast([128, NBLK, NEXP]), ALU.is_le), reads=[tq], rw=[tz])
            kb.op("dve", lambda e: e.tensor_reduce(be[:], T3[:], AX.X, ALU.add), rw=[tz])
            kb.op("dve", lambda e: e.tensor_scalar(be[:], be[:], float(NEXP - 1), 256.0, ALU.min, ALU.mult), rw=[tz])
            kb.op("dve", lambda e: e.tensor_tensor(idxw[:], be[:].unsqueeze(2).to_broadcast([128, NBLK, 2]),
                                                   hp[:].unsqueeze(1).to_broadcast([128, NBLK, 2]), ALU.add), reads=[tz, tq], rw=[t_rt])
            kb.barrier()

        if stop <= 5:
            return nc
        with ExitStack() as ph:
            wg = [sbuf(ph, "wg%d" % i, [128, 8 * DEXP], BF16) for i in range(2)]
            wu = [sbuf(ph, "wu%d" % i, [128, 8 * DEXP], BF16) for i in range(2)]
            wd = [sbuf(ph, "wd%d" % i, [128, 4 * D], BF16) for i in range(2)]
            xblk = [sbuf(ph, "xblk%d" % i, [128, 4, D], BF16) for i in range(2)]
            xT = sbuf(ph, "xT", [128, 8, 512], BF16)
            sg = [sbuf(ph, "sg%d" % i, [128, 512], BF16) for i in range(2)]
            actT = sbuf(ph, "actT", [128, 4, 512], BF16)
            ybuf = [sbuf(ph, "ybuf%d" % i, [128, 4, D], F32) for i in range(2)]
            x1t = [sbuf(ph, "x1t%d" % i, [128, D], F32) for i in range(4)]
            xs = [xblk[k][:, j, :] for k in range(2) for j in range(4)]
            y1 = [ybuf[0][:, j, :] for j in range(4)]
            y2 = [ybuf[1][:, j, :] for j in range(4)]
            gfB = sbuf(ph, "gfB", [128, D], F32)
            accf = sbuf(ph, "accf", [128, D], F32)
            junk = sbuf(ph, "junkE", [128, D], BF16)
            ssq = sbuf(ph, "ssqE", [128, 1], F32)
            ot = [sbuf(ph, "ot%d" % i, [128, D], F32) for i in range(2)]
            pT = [psum(ph, "pTE%d" % i, [128, 1024], BF16) for i in range(2)]
            pgt = [psum(ph, "pgt%d" % i, [128, 512], F32) for i in range(2)]
            put = [psum(ph, "put%d" % i, [128, 512], F32) for i in range(2)]
            py = [psum(ph, "py%d" % i, [128, 512], F32) for i in range(2)]
            t_zt, t_xs, t_wE, t_xb, t_xT = T(), [T() for _ in range(8)], [T(), T()], [T(), T()], T()
            t_sg, t_actT, t_yb, t_y1, t_y2, t_x1t = [T(), T()], T(), [T(), T()], [T() for _ in range(4)], [T() for _ in range(4)], [T() for _ in range(4)]
            t_gf, t_accf, t_junk, t_ssq, t_ot = T(), T(), T(), T(), [T(), T()]
            t_pT, t_pgt, t_put, t_py = [T(), T()], [T(), T()], [T(), T()], [T(), T()]
            kb.dma("sp", gfB[:], gfin.to_broadcast([128, D]), t_gf)
            XNs = XN.rearrange("(n p) d -> n p d", p=128)
            for ti in range(NS):
                b_ = ti % 8
                kb.dma("sp", xs[b_][:], XNs[ti], t_xs[b_], src=t_XN)
                for dsti in (dest1i, dest2i):
                    kb.dma("pool", None, None, t_XP, src=t_xs[b_], store=True, extra=[t_rt], waw=True,
                           fn=lambda e, dsti=dsti, ti=ti, b_=b_: e.indirect_dma_start(
                               out=XP[:, :], out_offset=bass.IndirectOffsetOnAxis(ap=dsti[:, ti:ti + 1], axis=0),
                               in_=xs[b_][:], in_offset=None))
            kb.barrier()
            XPb = XP.rearrange("(n s p) d -> n p s d", s=4, p=128)
            YPb = YP.rearrange("(n s p) d -> n p s d", s=4, p=128)
            cP, cY = [0], [0]

            def loadWb(b):
                bf = b % 2
                for (wt, wsrc) in ((wg, w_eg), (wu, w_eu), (wd, w_ed)):
                    for hf in range(2):
                        kb.dma("pool", None, None, t_wE[bf], extra=[t_rt],
                               fn=lambda e, wt=wt, wsrc=wsrc, hf=hf, bf=bf, b=b: e.indirect_dma_start(
                                   out=wt[bf][:, hf * 2048:(hf + 1) * 2048], out_offset=None, in_=wsrc[:, :],
                                   in_offset=bass.IndirectOffsetOnAxis(ap=idxw[:, b, hf:hf + 1], axis=0)))
                kb.dma("sp", xblk[bf][:], XPb[b], t_xb[bf], src=t_XP)

            loadWb(0)
            for b in range(NBLK):
                bf = b % 2
                if b + 1 < NBLK:
                    loadWb(b + 1)
                wgv = wg[bf][:].rearrange("p (kc f) -> p kc f", kc=8)
                wuv = wu[bf][:].rearrange("p (kc f) -> p kc f", kc=8)
                wdv = wd[bf][:].rearrange("p (fc d) -> p fc d", fc=4)
                for kc in range(8):
                    pb = kc % 2
                    for s in range(4):
                        kb.op("pe", lambda e, kc=kc, s=s, pb=pb, bf=bf: e.transpose(pT[pb][:, s * 128:(s + 1) * 128], xblk[bf][:, s, kc * 128:(kc + 1) * 128], ident_b[:]),
                              reads=[t_xb[bf], t_const], writes=[t_pT[pb]])
                    if kc % 2 == 0:
                        kb.op("act", lambda e, kc=kc, pb=pb: e.activation(xT[:, kc, :], pT[pb][:, 0:512], AF.Identity, scale=a2[:, kc:kc + 1], bias=b2[:, kc:kc + 1]),
                              reads=[t_pT[pb], t_mod], pw=[t_xT])
                    else:
                        kb.op("dve", lambda e, kc=kc, pb=pb: e.tensor_scalar(xT[:, kc, :], pT[pb][:, 0:512], a2[:, kc:kc + 1], b2[:, kc:kc + 1], ALU.mult, ALU.add),
                              reads=[t_pT[pb], t_mod], pw=[t_xT])
                for fc in range(4):
                    pb = cP[0] % 2
                    cP[0] += 1
                    for kc in range(8):
                        kb.op("pe", lambda e, kc=kc, fc=fc, pb=pb, wgv=wgv: e.matmul(pgt[pb][:, :], wgv[:, kc, fc * 128:(fc + 1) * 128], xT[:, kc, :],
                                                                                  start=(kc == 0), stop=(kc == 7)),
                              reads=[t_wE[bf], t_xT], writes=[t_pgt[pb]])
                    for kc in range(8):
                        kb.op("pe", lambda e, kc=kc, fc=fc, pb=pb, wuv=wuv: e.matmul(put[pb][:, :], wuv[:, kc, fc * 128:(fc + 1) * 128], xT[:, kc, :],
                                                                                  start=(kc == 0), stop=(kc == 7)),
                              reads=[t_wE[bf], t_xT], writes=[t_put[pb]])
                    kb.op("act", lambda e, pb=pb: e.activation(sg[pb][:], pgt[pb][:, :], AF.Silu), reads=[t_pgt[pb]], writes=[t_sg[pb]])
                    kb.op("dve", lambda e, pb=pb, fc=fc: e.tensor_tensor(actT[:, fc, :], sg[pb][:], put[pb][:, :], ALU.mult),
                          reads=[t_sg[pb], t_put[pb]], pw=[t_actT])
                for s in range(4):
                    for hf in range(2):
                        yb = cY[0] % 2
                        cY[0] += 1
                        for fc in range(4):
                            kb.op("pe", lambda e, fc=fc, s=s, hf=hf, yb=yb, wdv=wdv: e.matmul(py[yb][:, :], actT[:, fc, s * 128:(s + 1) * 128],
                                                                                           wdv[:, fc, hf * 512:(hf + 1) * 512], start=(fc == 0), stop=(fc == 3)),
                                  reads=[t_actT, t_wE[bf]], writes=[t_py[yb]])
                        if hf == 0:
                            kb.op("act", lambda e, yb=yb, s=s, hf=hf, bf=bf: e.activation(ybuf[bf][:, s, hf * 512:(hf + 1) * 512], py[yb][:, :], AF.Identity),
                                  reads=[t_py[yb]], pw=[t_yb[bf]])
                        else:
                            kb.op("dve", lambda e, yb=yb, s=s, hf=hf, bf=bf: e.tensor_copy(ybuf[bf][:, s, hf * 512:(hf + 1) * 512], py[yb][:, :]),
                                  reads=[t_py[yb]], pw=[t_yb[bf]])
                kb.dma("sp", YPb[b], ybuf[bf][:], t_YP, src=t_yb[bf], store=True)
            kb.barrier()
            X1s = X1.rearrange("(n p) d -> n p d", p=128)
            outs = out.rearrange("(n p) d -> n p d", p=128)
            for ti in range(NS):
                ob = ti % 4
                o2 = ti % 2
                kb.dma("pool", None, None, t_y1[ob], src=t_YP, extra=[t_rt],
                       fn=lambda e, ti=ti, ob=ob: e.indirect_dma_start(out=y1[ob][:], out_offset=None, in_=YP[:, :],
                                                                      in_offset=bass.IndirectOffsetOnAxis(ap=dest1i[:, ti:ti + 1], axis=0)))
                kb.dma("pool", None, None, t_y2[ob], src=t_YP, extra=[t_rt],
                       fn=lambda e, ti=ti, ob=ob: e.indirect_dma_start(out=y2[ob][:], out_offset=None, in_=YP[:, :],
                                                                      in_offset=bass.IndirectOffsetOnAxis(ap=dest2i[:, ti:ti + 1], axis=0)))
                kb.dma("sp", x1t[ob][:], X1s[ti], t_x1t[ob], src=t_X1)
                kb.op("act", lambda e, ti=ti, ob=ob: e.activation(accf[:], y1[ob][:], AF.Identity, scale=w1a[:, ti:ti + 1]), reads=[t_y1[ob], t_rt], writes=[t_accf])
                kb.op("dve", lambda e, ti=ti, ob=ob: e.scalar_tensor_tensor(accf[:], y2[ob][:], w2a[:, ti:ti + 1], accf[:], ALU.mult, ALU.add),
                      reads=[t_y2[ob], t_rt], rw=[t_accf])
                kb.op("dve", lambda e: e.tensor_tensor(accf[:], accf[:], gate2B[:], ALU.mult), reads=[t_mod], rw=[t_accf])
                kb.op("dve", lambda e, ob=ob: e.tensor_tensor(accf[:], accf[:], x1t[ob][:], ALU.add), reads=[t_x1t[ob]], rw=[t_accf])
                kb.op("act", lambda e: e.activation(junk[:], accf[:], AF.Square, accum_out=ssq[:]), reads=[t_accf], writes=[t_junk, t_ssq])
                kb.op("act", lambda e: e.activation(ssq[:], ssq[:], AF.Sqrt, scale=1.0 / D, bias=EPS), rw=[t_ssq])
                kb.op("dve", lambda e: e.reciprocal(ssq[:], ssq[:]), rw=[t_ssq])
                kb.op("dve", lambda e, o2=o2: e.scalar_tensor_tensor(ot[o2][:], accf[:], ssq[:, 0:1], gfB[:], ALU.mult, ALU.mult),
                      reads=[t_accf, t_ssq, t_gf], writes=[t_ot[o2]])
                kb.dma("sp", outs[ti], ot[o2][:], t_out, src=t_ot[o2], store=True)
            kb.barrier()
    return nc


def _consts():
    ident = np.eye(128, dtype=np.float32)
    tri = np.triu(np.ones((128, 128), np.float32))
    sel4 = np.zeros((4, 4, 128), np.float32)
    for h in range(4):
        sel4[h, h, :] = 1.0
    trs = np.triu(np.ones((128, 128), np.float32), k=1)
    hp = np.stack([np.arange(128, dtype=np.float32), 128.0 + np.arange(128, dtype=np.float32)], axis=1)
    return ident, tri, sel4, trs, hp


def prep_core_inputs(inp, b):
    f = np.float32
    ident, tri, sel4, trs, hp = _consts()
    S = np.asarray(inp["x"]).shape[1]
    nblk = (2 * S) // 512 + NEXP
    thr = np.ascontiguousarray(np.broadcast_to(512.0 * np.arange(nblk, dtype=np.float32), (128, nblk)))

    def pl(v):
        return np.ascontiguousarray(np.asarray(v, f).reshape(8, 128).T)

    bg = np.asarray(inp["b_gates"], f)[0]
    wconv = np.asarray(inp["w_conv"], f)[0]
    m = {
        "x": np.ascontiguousarray(np.asarray(inp["x"], f)[b]),
        "c_l": pl(np.asarray(inp["c"], f)[b]),
        "w_ada": np.ascontiguousarray(np.asarray(inp["w_ada"], f)[0]),
        "b_ada": np.ascontiguousarray(np.asarray(inp["b_ada"], f)[0].reshape(1, -1)),
        "g1_l": pl(inp["g_norm1"][0]),
        "g2_l": pl(inp["g_norm2"][0]),
        "gfin": np.ascontiguousarray(np.asarray(inp["g_final"], f).reshape(1, -1)),
        "w_in": np.ascontiguousarray(np.asarray(inp["w_in"], f)[0]),
        "wconv_l": np.ascontiguousarray(wconv.reshape(4, 8, 128).transpose(2, 1, 0)),
        "bconv_l": pl(inp["b_conv"][0]),
        "bgi": np.ascontiguousarray(bg[0:4].reshape(4, 1)),
        "bgf": np.ascontiguousarray(bg[4:8].reshape(4, 1)),
        "gml": np.ascontiguousarray(np.asarray(inp["g_mlstm_head"], f)[0].reshape(1, -1)),
        "w_out": np.ascontiguousarray(np.asarray(inp["w_out"], f)[0]),
        "w_r": np.ascontiguousarray(np.concatenate([np.asarray(inp["w_router_group"], f)[0],
                                                    np.asarray(inp["w_router_expert"], f)[0]], axis=1)),
        "b_r": np.ascontiguousarray(np.concatenate([np.asarray(inp["b_router_group"], f)[0],
                                                    np.asarray(inp["b_router_expert"], f)[0]]).reshape(1, -1)),
        "ident": ident, "tri": tri, "sel4": sel4, "trs": trs, "hp": hp, "thr": thr,
    }
    return m


def prep_shared(inp):
    f = np.float32
    def lay(w, n):
        C = w.shape[2]
        return np.ascontiguousarray(w.reshape(NEXP, 2, n, 128, C).transpose(0, 1, 3, 2, 4).reshape(NEXP * 2 * 128, n * C))
    return {
        "w_eg": lay(np.asarray(inp["w_expert_gate"], f)[0], 4),
        "w_eu": lay(np.asarray(inp["w_expert_up"], f)[0], 4),
        "w_ed": lay(np.asarray(inp["w_expert_down"], f)[0], 2),
    }


def kernel(**inputs):
    B, S, _ = inputs["x"].shape
    nc = build(S)
    shared = prep_shared(inputs)
    in_maps = [dict(prep_core_inputs(inputs, b), **shared) for b in range(B)]
    res = run_bass_kernel_spmd(nc, in_maps, core_ids=list(range(B)))
    return np.stack([np.asarray(r["out"], np.float32) for r in res.results], axis=0)
```

```python
import os
import numpy as np
from contextlib import ExitStack
import concourse.bass as bass
import concourse.mybir as mybir
from concourse.bass_utils import run_bass_kernel_spmd

F32 = mybir.dt.float32
BF16 = mybir.dt.bfloat16
I32 = mybir.dt.int32
AF = mybir.ActivationFunctionType
ALU = mybir.AluOpType
AX = mybir.AxisListType

D = 1024
INC = 3592
EPS = 1e-6
NEXP = 32
DEXP = 512
BIG = 1.0e30


class Tok:
    __slots__ = ("name", "writes", "xwrites", "reads", "sem", "total")

    def __init__(self, name=""):
        self.name = name
        self.writes = {}
        self.xwrites = {}
        self.reads = {}
        self.sem = None
        self.total = 0


class KB:
    def __init__(self, nc, stack):
        self.nc = nc
        self.stack = stack
        self.eng = {"pe": nc.tensor, "dve": nc.vector, "act": nc.scalar, "pool": nc.gpsimd, "sp": nc.sync}
        self.sems = {}
        self.cnt = {}
        self.semobj = {}
        for e in self.eng:
            s = stack.enter_context(nc.semaphore("s_" + e))
            self.sems[e] = s
            self.semobj[("e", e)] = s
            self.cnt[e] = 0
        self.waited = {e: {} for e in self.eng}
        self.ntok = 0
        self.dtoks = []

    def tok(self, name=""):
        return Tok(name)

    def _toksem(self, t, kind):
        if t.sem is None:
            t.sem = {}
            t.total = {}
            self.dtoks.append(t)
        if kind not in t.sem:
            self.ntok += 1
            t.sem[kind] = self.stack.enter_context(self.nc.semaphore("d%d" % self.ntok))
            t.total[kind] = 0
            self.semobj[("t", id(t), kind)] = t.sem[kind]
        return ("t", id(t), kind)

    def _wait(self, e, deps):
        w = self.waited[e]
        for k, v in deps.items():
            if k == ("e", "pe") and e == "pe":
                continue
            if w.get(k, 0) >= v:
                continue
            self.eng[e].wait_ge(self.semobj[k], v)
            w[k] = v

    @staticmethod
    def _merge(d, src):
        for k, v in src.items():
            if d.get(k, 0) < v:
                d[k] = v

    def op(self, e, fn, reads=(), writes=(), rw=(), pw=()):
        deps = {}
        for t in list(reads) + list(rw):
            self._merge(deps, t.writes)
        for t in list(writes) + list(rw):
            self._merge(deps, t.writes)
            self._merge(deps, t.reads)
        for t in pw:
            self._merge(deps, t.xwrites)
            self._merge(deps, t.reads)
        self._wait(e, deps)
        ins = fn(self.eng[e])
        self.cnt[e] += 1
        v = self.cnt[e]
        ins.then_inc(self.sems[e], 1)
        k = ("e", e)
        for t in reads:
            t.reads[k] = v
        for t in list(writes) + list(rw):
            t.writes[k] = v
            t.xwrites[k] = v
        for t in pw:
            t.writes[k] = v
        return ins

    def dma(self, q, out, in_, dst, src=None, store=False, fn=None, extra=(), waw=False):
        deps = {}
        for t in ([src] if src is not None else []) + list(extra):
            for k, v in t.writes.items():
                if deps.get(k, 0) < v:
                    deps[k] = v
        for k, v in dst.reads.items():
            if deps.get(k, 0) < v:
                deps[k] = v
        for k, v in dst.writes.items():
            if (waw or k[0] == "e") and deps.get(k, 0) < v:
                deps[k] = v
        self._wait(q, deps)
        kind = "sw" if q == "pool" else "hw"
        own = src if store else dst
        key = self._toksem(own, kind)
        if fn is None:
            ins = self.eng[q].dma_start(out=out, in_=in_)
        else:
            ins = fn(self.eng[q])
        own.total[kind] += 16
        ins.then_inc(own.sem[kind], 16)
        dst.writes[key] = own.total[kind]
        dst.xwrites[key] = own.total[kind]
        for t in ([src] if src is not None else []) + list(extra):
            t.reads[key] = own.total[kind]
        return ins

    def wait_tok(self, e, t):
        self._wait(e, dict(t.writes))

    def barrier(self):
        deps = {("e", e): self.cnt[e] for e in self.eng if self.cnt[e] > 0}
        for t in self.dtoks:
            for kind, tot in t.total.items():
                deps[("t", id(t), kind)] = tot
        for e in self.eng:
            d = {k: v for k, v in deps.items() if k != ("e", e)}
            self._wait(e, d)


def build(S, dbg=False, stop=99):
    nc = bass.Bass("TRN2", target_bir_lowering=False)
    NT = S // 512
    NB = S // 256
    NCH = S // 64
    NS = S // 128
    assert NCH <= 128

    def din(name, shape, dt=F32):
        return nc.dram_tensor(name, shape, dt, kind="ExternalInput").ap()

    skind = "ExternalOutput" if dbg else "Internal"

    def dscr(name, shape, dt):
        return nc.dram_tensor(name, shape, dt, kind=skind).ap()

    x = din("x", [S, D])
    c_l = din("c_l", [128, 8])
    w_ada = din("w_ada", [D, 6 * D])
    b_ada = din("b_ada", [1, 6 * D])
    g1_l = din("g1_l", [128, 8])
    g2_l = din("g2_l", [128, 8])
    gfin = din("gfin", [1, D])
    w_in = din("w_in", [D, INC])
    wconv_l = din("wconv_l", [128, 8, 4])
    bconv_l = din("bconv_l", [128, 8])
    bgi = din("bgi", [4, 1])
    bgf = din("bgf", [4, 1])
    gml = din("gml", [1, 512])
    w_out = din("w_out", [D, D])
    w_r = din("w_r", [D, 36])
    b_r = din("b_r", [1, 36])
    w_eg = din("w_eg", [NEXP * 2 * 128, 2048])
    w_eu = din("w_eu", [NEXP * 2 * 128, 2048])
    w_ed = din("w_ed", [NEXP * 2 * 128, 2048])
    NBLK = (2 * S) // 512 + NEXP
    trs_d = din("trs", [128, 128])
    thr_d = din("thr", [128, NBLK])
    hp_d = din("hp", [128, 2])
    ident_d = din("ident", [128, 128])
    tri_d = din("tri", [128, 128])
    sel4_d = din("sel4", [4, 4, 128])
    out = nc.dram_tensor("out", [S, D], F32, kind="ExternalOutput").ap()

    FM = dscr("FM", [16, 128, S], BF16)
    TM = dscr("TM", [S, 1536], BF16)
    G = dscr("G", [8, S], F32)
    HC = dscr("HC", [S, D], BF16)
    X1 = dscr("X1", [S, D], F32)
    XN = dscr("XN", [S, D], BF16)
    XP = dscr("XP", [NBLK * 512, D], BF16)
    YP = dscr("YP", [NBLK * 512, D], F32)

    with ExitStack() as st:
        kb = KB(nc, st)
        T = kb.tok

        def sbuf(ctx, name, shape, dt):
            return ctx.enter_context(nc.sbuf_tensor(name, shape, dt))

        def psum(ctx, name, shape, dt):
            return ctx.enter_context(nc.psum_tensor(name, shape, dt))

        ident_f = sbuf(st, "ident_f", [128, 128], F32)
        ident_b = sbuf(st, "ident_b", [128, 128], BF16)
        tri_b = sbuf(st, "tri_b", [128, 128], BF16)
        ones_f = sbuf(st, "ones_f", [128, 128], F32)
        a1 = sbuf(st, "a1", [128, 8], F32)
        b1 = sbuf(st, "b1", [128, 8], F32)
        a2 = sbuf(st, "a2", [128, 8], F32)
        b2 = sbuf(st, "b2", [128, 8], F32)
        gate1B = sbuf(st, "gate1B", [128, D], F32)
        gate2B = sbuf(st, "gate2B", [128, D], F32)
        OH1a = sbuf(st, "OH1a", [128, NS, NEXP], F32)
        OH2a = sbuf(st, "OH2a", [128, NS, NEXP], F32)
        w1a = sbuf(st, "w1a", [128, NS], F32)
        w2a = sbuf(st, "w2a", [128, NS], F32)
        dest1i = sbuf(st, "dest1i", [128, NS], I32)
        dest2i = sbuf(st, "dest2i", [128, NS], I32)
        idxw = sbuf(st, "idxw", [128, NBLK, 2], I32)
        t_const = T("const")
        t_mod = T("mod")
        t_cw = T("cw")
        t_FM, t_TM, t_G, t_HC, t_X1, t_XN, t_out = T("FM"), T("TM"), T("G"), T("HC"), T("X1"), T("XN"), T("out")
        t_XP, t_YP, t_rt = T("XP"), T("YP"), T("route")

        kb.dma("sp", ident_f[:], ident_d[:], t_const)
        kb.dma("pool", ident_b[:], ident_d[:], t_const)
        kb.dma("pool", tri_b[:], tri_d[:], t_const)
        t_ones = T("ones")
        kb.op("pool", lambda e: e.memset(ones_f[:], 1.0), writes=[t_ones])

        ph0A = ExitStack()
        w_in_sb = sbuf(ph0A, "w_in_sb", [128, 8, INC], BF16)
        t_w = T()
        w_in_v = w_in.rearrange("(kc p) n -> p kc n", p=128)
        for hf in range(2):
            kb.dma("pool", w_in_sb[:, :, hf * 1796:(hf + 1) * 1796], w_in_v[:, :, hf * 1796:(hf + 1) * 1796], t_w)

        with ExitStack() as ph:
            c_sb = sbuf(ph, "c_sb", [128, 8], F32)
            sc = sbuf(ph, "sc", [128, 8], F32)
            g1s = sbuf(ph, "g1s", [128, 8], F32)
            g2s = sbuf(ph, "g2s", [128, 8], F32)
            modrow = sbuf(ph, "modrow", [1, 6 * D], F32)
            brow = sbuf(ph, "brow", [1, 6 * D], F32)
            modT = sbuf(ph, "modT", [128, 48], F32)
            wa = [sbuf(ph, "wa%d" % i, [128, 8, 512], F32) for i in range(2)]
            ps0 = psum(ph, "ps0", [128, 512], F32)
            ps1 = psum(ph, "ps1", [128, 512], F32)
            t_c, t_sc, t_g, t_brow, t_modrow = T(), T(), T(), T(), T()
            t_wa = [T(), T()]
            t_ps0, t_ps1, t_modT = T(), T(), T()
            kb.dma("sp", c_sb[:], c_l[:], t_c)
            kb.dma("sp", g1s[:], g1_l[:], t_g)
            kb.dma("sp", g2s[:], g2_l[:], t_g)
            kb.dma("sp", brow[:], b_ada[:], t_brow)
            kb.op("act", lambda e: e.activation(sc[:], c_sb[:], AF.Silu), reads=[t_c], writes=[t_sc])
            wav = w_ada.rearrange("(kc p) n -> p kc n", p=128)
            for blk in range(12):
                bf = blk % 2
                kb.dma("sp", wa[bf][:], wav[:, :, blk * 512:(blk + 1) * 512], t_wa[bf])
                for kc in range(8):
                    kb.op("pe", lambda e, kc=kc, bf=bf: e.matmul(ps0[0:1, :], sc[:, kc:kc + 1], wa[bf][:, kc, :],
                                                               start=(kc == 0), stop=(kc == 7)),
                          reads=[t_sc, t_wa[bf]], writes=[t_ps0])
                kb.op("dve", lambda e, blk=blk: e.tensor_tensor(modrow[0:1, blk * 512:(blk + 1) * 512], ps0[0:1, :],
                                                                brow[0:1, blk * 512:(blk + 1) * 512], ALU.add),
                      reads=[t_ps0, t_brow], writes=[t_modrow])
            for (gB, c0) in ((gate1B, 2 * D), (gate2B, 5 * D)):
                for hf in range(2):
                    kb.op("pe", lambda e, c0=c0, hf=hf: e.matmul(ps1[:, :], ones_f[0:1, :], modrow[0:1, c0 + hf * 512:c0 + (hf + 1) * 512],
                                                                 start=True, stop=True),
                          reads=[t_modrow, t_ones], writes=[t_ps1])
                    kb.op("act", lambda e, gB=gB, hf=hf: e.activation(gB[:, hf * 512:(hf + 1) * 512], ps1[:, :], AF.Identity),
                          reads=[t_ps1], writes=[t_mod])
            for j in range(48):
                kb.op("pe", lambda e, j=j: e.matmul(ps0[:, j:j + 1], modrow[0:1, j * 128:(j + 1) * 128], ones_f[0:1, 0:1],
                                                    start=True, stop=True),
                      reads=[t_modrow, t_ones], writes=[t_ps0])
            kb.op("dve", lambda e: e.tensor_copy(modT[:], ps0[:, 0:48]), reads=[t_ps0], writes=[t_modT])
            kb.op("dve", lambda e: e.scalar_tensor_tensor(a1[:], modT[:, 8:16], 1.0, g1s[:], ALU.add, ALU.mult),
                  reads=[t_modT, t_g], writes=[t_mod])
            kb.op("dve", lambda e: e.tensor_copy(b1[:], modT[:, 0:8]), reads=[t_modT], writes=[t_mod])
            kb.op("dve", lambda e: e.scalar_tensor_tensor(a2[:], modT[:, 32:40], 1.0, g2s[:], ALU.add, ALU.mult),
                  reads=[t_modT, t_g], writes=[t_mod])
            kb.op("dve", lambda e: e.tensor_copy(b2[:], modT[:, 24:32]), reads=[t_modT], writes=[t_mod])
            kb.barrier()
        if stop <= 0:
            return nc

        with ExitStack() as ph:
            wc = sbuf(ph, "wc", [128, 8, 4], F32)
            bc = sbuf(ph, "bc", [128, 8], F32)
            bi_s = sbuf(ph, "bi_s", [4, 1], F32)
            bf_s = sbuf(ph, "bf_s", [4, 1], F32)
            xt = [sbuf(ph, "xt%d" % i, [128, 4, D], F32) for i in range(2)]
            junk = sbuf(ph, "junk", [128, D], BF16)
            ssq = sbuf(ph, "ssq", [128, 4], F32)
            rs = sbuf(ph, "rs", [128, 4], F32)
            xn2 = [sbuf(ph, "xn%d" % i, [128, 4, D], BF16) for i in range(2)]
            hnT = [sbuf(ph, "hnT%d" % i, [128, 8, 512], BF16) for i in range(2)]
            convbuf = sbuf(ph, "convbuf", [128, 8, 515], F32)
            cacc = [sbuf(ph, "cacc%d" % i, [128, 512], F32) for i in range(2)]
            sgt = [sbuf(ph, "sgt%d" % i, [128, 512], F32) for i in range(2)]
            stage_m = sbuf(ph, "stage_m", [128, 8, 512], BF16)
            stage_a = sbuf(ph, "stage_a", [128, 8, 512], BF16)
            stage_t = sbuf(ph, "stage_t", [128, 4, 1536], BF16)
            gst = sbuf(ph, "gst", [4, 2, 512], F32)
            pT = [psum(ph, "pT%d" % i, [128, 1024], BF16) for i in range(2)]
            pm = [psum(ph, "pm%d" % i, [128, 512], F32) for i in range(4)]
            pg = [psum(ph, "pg%d" % i, [4, 512], F32) for i in range(2)]
            t_wc = T()
            t_xt = [T(), T()]
            t_junk, t_ssq, t_rs, t_xn2 = T(), T(), T(), [T(), T()]
            t_hnT = [T(), T()]
            t_pT = [T(), T()]
            t_pm = [T() for _ in range(4)]
            t_pg = [T(), T()]
            t_conv = [T() for _ in range(8)]
            t_cacc = [T(), T()]
            t_sgt = [T(), T()]
            t_sm, t_sa, t_stt, t_gst = T(), T(), T(), T()
            kb.dma("sp", wc[:], wconv_l[:], t_wc)
            kb.dma("sp", bc[:], bconv_l[:], t_wc)
            kb.dma("sp", bi_s[:], bgi[:], t_wc)
            kb.dma("sp", bf_s[:], bgf[:], t_wc)
            kb.op("dve", lambda e: e.memset(convbuf[:, :, 0:3], 0.0), writes=t_conv)
            xv = x.rearrange("(n s p) d -> n p s d", s=4, p=128)
            FMv = FM.rearrange("g p t -> p g t")
            TMv = TM.rearrange("(n s p) c -> n p s c", s=4, p=128)
            kb.dma("sp", xt[0][:], xv[0], t_xt[0])
            pmi = [0]

            def next_pm():
                i = pmi[0] % 4
                pmi[0] += 1
                return i

            def normA(i):
                cur = i % 2
                xc = xt[cur]
                for s in range(4):
                    kb.op("act", lambda e, s=s, xc=xc: e.activation(junk[:], xc[:, s, :], AF.Square, accum_out=ssq[:, s:s + 1]),
                          reads=[t_xt[cur]], writes=[t_junk, t_ssq])
                kb.op("act", lambda e: e.activation(rs[:], ssq[:], AF.Sqrt, scale=1.0 / D, bias=EPS), reads=[t_ssq], writes=[t_rs])
                kb.op("dve", lambda e: e.reciprocal(rs[:], rs[:]), rw=[t_rs])
                for s in range(4):
                    kb.op("dve", lambda e, s=s, xc=xc, cur=cur: e.tensor_scalar(xn2[cur][:, s, :], xc[:, s, :], rs[:, s:s + 1], None, ALU.mult),
                          reads=[t_xt[cur], t_rs], pw=[t_xn2[cur]])

            normA(0)
            for i in range(NT):
                cur = i % 2
                if i + 1 < NT:
                    kb.dma("sp", xt[1 - cur][:], xv[i + 1], t_xt[1 - cur])
                xn, t_xn = xn2[cur], t_xn2[cur]
                hc = hnT[cur]
                for kc in range(8):
                    pb = kc % 2
                    for s in range(4):
                        kb.op("pe", lambda e, kc=kc, s=s, pb=pb, xn=xn: e.transpose(pT[pb][:, s * 128:(s + 1) * 128],
                                                                              xn[:, s, kc * 128:(kc + 1) * 128], ident_b[:]),
                              reads=[t_xn, t_const], writes=[t_pT[pb]])
                    if kc % 2 == 0:
                        kb.op("act", lambda e, kc=kc, pb=pb: e.activation(hc[:, kc, :], pT[pb][:, 0:512], AF.Identity,
                                                                          scale=a1[:, kc:kc + 1], bias=b1[:, kc:kc + 1]),
                              reads=[t_pT[pb], t_mod], pw=[t_hnT[cur]])
                    else:
                        kb.op("dve", lambda e, kc=kc, pb=pb: e.tensor_scalar(hc[:, kc, :], pT[pb][:, 0:512], a1[:, kc:kc + 1],
                                                                             b1[:, kc:kc + 1], ALU.mult, ALU.add),
                              reads=[t_pT[pb], t_mod], pw=[t_hnT[cur]])
                if i + 1 < NT:
                    normA(i + 1)
                for g in range(0, 8):
                    if g < 8:
                        c0 = g * 128
                    elif g < 12:
                        c0 = 2056 + (g - 8) * 128
                    else:
                        c0 = 2568 + (g - 12) * 128
                    pi = next_pm()
                    for kc in range(8):
                        kb.op("pe", lambda e, kc=kc, c0=c0, pi=pi: e.matmul(pm[pi][:, :], w_in_sb[:, kc, c0:c0 + 128], hc[:, kc, :],
                                                                          start=(kc == 0), stop=(kc == 7)),
                              reads=[t_w, t_hnT[cur]], writes=[t_pm[pi]])
                    if g < 8:
                        kb.op("act", lambda e, g=g, pi=pi: e.activation(convbuf[:, g, 3:515], pm[pi][:, :], AF.Identity),
                              reads=[t_pm[pi]], writes=[t_conv[g]])
                    elif g < 12:
                        kb.op("act", lambda e, g=g, pi=pi: e.activation(stage_a[:, g - 8, :], pm[pi][:, :], AF.Identity, scale=0.125),
                              reads=[t_pm[pi]], pw=[t_sa])
                    else:
                        kb.op("act", lambda e, g=g, pi=pi: e.activation(stage_a[:, g - 8, :], pm[pi][:, :], AF.Identity),
                              reads=[t_pm[pi]], pw=[t_sa])
                for g in range(8 if 'conv' not in os.environ.get('KSKIP', '') else 0):
                    cb = g % 2
                    kb.op("dve", lambda e, g=g, cb=cb: e.tensor_scalar(cacc[cb][:], convbuf[:, g, 0:512], wc[:, g, 0:1], bc[:, g:g + 1],
                                                                       ALU.mult, ALU.add),
                          reads=[t_conv[g], t_wc], writes=[t_cacc[cb]])
                    for j in range(1, 4):
                        kb.op("dve", lambda e, g=g, cb=cb, j=j: e.scalar_tensor_tensor(cacc[cb][:], convbuf[:, g, j:j + 512], wc[:, g, j:j + 1],
                                                                                      cacc[cb][:], ALU.mult, ALU.add),
                              reads=[t_conv[g], t_wc], rw=[t_cacc[cb]])
                    kb.op("act", lambda e, cb=cb: e.activation(sgt[cb][:], cacc[cb][:], AF.Sigmoid), reads=[t_cacc[cb]], writes=[t_sgt[cb]])
                    qs = (128.0 ** -0.5) if g < 4 else 1.0
                    kb.op("dve", lambda e, g=g, cb=cb, qs=qs: e.scalar_tensor_tensor(stage_m[:, g, :], sgt[cb][:], qs, cacc[cb][:],
                                                                                   ALU.mult, ALU.mult),
                          reads=[t_sgt[cb], t_cacc[cb]], pw=[t_sm])
                    kb.op("dve", lambda e, g=g: e.tensor_copy(convbuf[:, g, 0:3], convbuf[:, g, 512:515]), rw=[t_conv[g]])
                for g in range(8, 16):
                    if g < 8:
                        c0 = g * 128
                    elif g < 12:
                        c0 = 2056 + (g - 8) * 128
                    else:
                        c0 = 2568 + (g - 12) * 128
                    pi = next_pm()
                    for kc in range(8):
                        kb.op("pe", lambda e, kc=kc, c0=c0, pi=pi: e.matmul(pm[pi][:, :], w_in_sb[:, kc, c0:c0 + 128], hc[:, kc, :],
                                                                          start=(kc == 0), stop=(kc == 7)),
                              reads=[t_w, t_hnT[cur]], writes=[t_pm[pi]])
                    if g < 8:
                        kb.op("act", lambda e, g=g, pi=pi: e.activation(convbuf[:, g, 3:515], pm[pi][:, :], AF.Identity),
                              reads=[t_pm[pi]], writes=[t_conv[g]])
                    elif g < 12:
                        kb.op("act", lambda e, g=g, pi=pi: e.activation(stage_a[:, g - 8, :], pm[pi][:, :], AF.Identity, scale=0.125),
                              reads=[t_pm[pi]], pw=[t_sa])
                    else:
                        kb.op("act", lambda e, g=g, pi=pi: e.activation(stage_a[:, g - 8, :], pm[pi][:, :], AF.Identity),
                              reads=[t_pm[pi]], pw=[t_sa])
                for gi, (c0, bs) in enumerate(((2048, bi_s), (2052, bf_s)) if 'gates' not in os.environ.get('KSKIP', '') else ()):
                    for kc in range(8):
                        kb.op("pe", lambda e, kc=kc, c0=c0, gi=gi: e.matmul(pg[gi][:, :], w_in_sb[:, kc, c0:c0 + 4], hc[:, kc, :],
                                                                          start=(kc == 0), stop=(kc == 7)),
                              reads=[t_w, t_hnT[cur]], writes=[t_pg[gi]])
                    kb.op("act", lambda e, gi=gi, bs=bs: e.activation(gst[:, gi, :], pg[gi][:, :], AF.Identity, bias=bs[:, 0:1]),
                          reads=[t_pg[gi], t_wc], pw=[t_gst])
                for s in range(4 if 'tm' not in os.environ.get('KSKIP', '') else 0):
                    for cg, c0 in enumerate((1024, 1536, 3080)):
                        pi = next_pm()
                        for kc in range(8):
                            kb.op("pe", lambda e, kc=kc, c0=c0, pi=pi, s=s: e.matmul(pm[pi][:, :], hc[:, kc, s * 128:(s + 1) * 128],
                                                                                   w_in_sb[:, kc, c0:c0 + 512],
                                                                                   start=(kc == 0), stop=(kc == 7)),
                                  reads=[t_w, t_hnT[cur]], writes=[t_pm[pi]])
                        if cg == 1:
                            kb.op("act", lambda e, s=s, pi=pi: e.activation(stage_t[:, s, 512:1024], pm[pi][:, :], AF.Sigmoid),
                                  reads=[t_pm[pi]], pw=[t_stt])
                        elif cg == 0:
                            kb.op("act", lambda e, s=s, pi=pi: e.activation(stage_t[:, s, 0:512], pm[pi][:, :], AF.Identity),
                                  reads=[t_pm[pi]], pw=[t_stt])
                        else:
                            kb.op("act", lambda e, s=s, pi=pi: e.activation(stage_t[:, s, 1024:1536], pm[pi][:, :], AF.Identity),
                                  reads=[t_pm[pi]], pw=[t_stt])
                kb.dma("sp", FMv[:, 0:8, i * 512:(i + 1) * 512], stage_m[:], t_FM, src=t_sm, store=True)
                kb.dma("sp", FMv[:, 8:16, i * 512:(i + 1) * 512], stage_a[:], t_FM, src=t_sa, store=True)
                kb.dma("sp", TMv[i], stage_t[:], t_TM, src=t_stt, store=True)
                kb.dma("sp", G[0:4, i * 512:(i + 1) * 512], gst[:, 0, :], t_G, src=t_gst, store=True)
                kb.dma("sp", G[4:8, i * 512:(i + 1) * 512], gst[:, 1, :], t_G, src=t_gst, store=True)
            kb.barrier()
        ph0A.close()

        if stop <= 1:
            return nc
        with ExitStack() as ph:
            gi_c = sbuf(ph, "gi_c", [NCH, 4, 64], F32)
            gf_c = sbuf(ph, "gf_c", [NCH, 4, 64], F32)
            tmpg = sbuf(ph, "tmpg", [NCH, 4, 64], F32)
            bneg = sbuf(ph, "bneg", [NCH, 4, 64], F32)
            a_t = sbuf(ph, "a_t", [NCH, 4, 64], F32)
            u_t = sbuf(ph, "u_t", [NCH, 4, 64], F32)
            fl_t = sbuf(ph, "fl_t", [NCH, 4, 64], F32)
            amax = sbuf(ph, "amax", [NCH, 4], F32)
            blast = sbuf(ph, "blast", [NCH, 4], F32)
            negM = sbuf(ph, "negM", [NCH, 4], F32)
            amaxT = sbuf(ph, "amaxT", [4, NCH], F32)
            blastT = sbuf(ph, "blastT", [4, NCH], F32)
            mrow = sbuf(ph, "mrow", [4, NCH], F32)
            Mend = sbuf(ph, "Mend", [4, NCH], F32)
            mprev = sbuf(ph, "mprev", [4, NCH], F32)
            wold = sbuf(ph, "wold", [4, NCH], F32)
            uT = sbuf(ph, "uT", [64, 4, NCH], F32)
            flT = sbuf(ph, "flT", [64, 4, NCH], F32)
            wb = sbuf(ph, "wb", [128, 4, NCH], F32)
            sel4s = sbuf(ph, "sel4s", [4, 4, 128], F32)
            gmB = sbuf(ph, "gmB", [64, 512], F32)
            mask64 = sbuf(ph, "mask64", [64, 64], F32)
            qk = [sbuf(ph, "qk%d" % i, [128, 8, 512], BF16) for i in range(2)]
            vt = [sbuf(ph, "vt%d" % i, [64, 8, 4, 129], BF16) for i in range(2)]
            gso = [sbuf(ph, "gso%d" % i, [64, 8, 512], BF16) for i in range(2)]
            accS = [sbuf(ph, "accS%d" % i, [64, 4, 129], F32) for i in range(2)]
            sa_2 = [sbuf(ph, "sa%d" % i, [64, 4, 1], F32) for i in range(2)]
            tq_2 = [sbuf(ph, "tq%d" % i, [64, 4, 1], F32) for i in range(2)]
            junkS = sbuf(ph, "junkS", [64, 128], BF16)
            so = [sbuf(ph, "so%d" % i, [64, 8, 512], BF16) for i in range(2)]
            C32 = [sbuf(ph, "C32_%d" % i, [128, 4, 129], F32) for i in range(2)]
            Csb = sbuf(ph, "Csb", [128, 4, 129], BF16)
            ktok_2 = [sbuf(ph, "ktok_%d" % i, [64, 4, 128], BF16) for i in range(2)]
            vp_2 = [sbuf(ph, "vp_%d" % i, [64, 4, 130], BF16) for i in range(2)]
            scT_2 = [sbuf(ph, "scT_%d" % i, [64, 4, 64], BF16) for i in range(2)]
            den_2 = [sbuf(ph, "den_%d" % i, [64, 4, 1], F32) for i in range(2)]
            hb_2 = [sbuf(ph, "hb_%d" % i, [64, 4, 128], F32) for i in range(2)]
            hsq_2 = [sbuf(ph, "hsq_%d" % i, [64, 4, 128], F32) for i in range(2)]
            ssq2_2 = [sbuf(ph, "ssq2_%d" % i, [64, 4, 1], F32) for i in range(2)]
            hb2_2 = [sbuf(ph, "hb2_%d" % i, [64, 4, 128], F32) for i in range(2)]
            ostage = [sbuf(ph, "ostage%d" % i, [64, 8, 512], BF16) for i in range(2)]
            psA = psum(ph, "psA", [128, 512], F32)
            pk = psum(ph, "pk", [64, 1024], BF16)
            pp = psum(ph, "pp", [64, 512], F32)
            pacc = psum(ph, "pacc", [64, 1024], F32)
            pc = psum(ph, "pc", [128, 1024], F32)
            paccv = pacc[:].rearrange("p (h e) -> p h e", h=4)
            pcv = pc[:].rearrange("p (h e) -> p h e", h=4)
            pkv = pk[:, 0:512].rearrange("p (h e) -> p h e", h=4)
            ppv = pp[:, 0:256].rearrange("p (h e) -> p h e", h=4)
            tg = T()
            t_psA = T()
            kb.dma("sp", gi_c[:], G[0:4, :].rearrange("h (c l) -> c h l", l=64), tg, src=t_G)
            kb.dma("sp", gf_c[:], G[4:8, :].rearrange("h (c l) -> c h l", l=64), tg, src=t_G)
            kb.dma("sp", sel4s[:], sel4_d[:], tg)
            kb.dma("sp", gmB[:], gml.to_broadcast([64, 512]), tg)
            kb.dma("sp", mask64[:], tri_d[0:64, 0:64], tg)
            tp = T()
            kb.op("act", lambda e: e.activation(tmpg[:], gf_c[:], AF.Exp, scale=-1.0), reads=[tg], writes=[tp])
            kb.op("act", lambda e: e.activation(tmpg[:], tmpg[:], AF.Ln, bias=1.0), rw=[tp])
            for h in range(4):
                kb.op("dve", lambda e, h=h: e.tensor_tensor_scan(bneg[:, h, :], ones_f[0:NCH, 0:64], tmpg[:, h, :], 0.0, ALU.mult, ALU.add),
                      reads=[t_ones], rw=[tp])
            kb.op("dve", lambda e: e.tensor_tensor(a_t[:], gi_c[:], bneg[:], ALU.add), reads=[tg], rw=[tp])
            kb.op("dve", lambda e: e.tensor_reduce(amax[:], a_t[:], AX.X, ALU.max), rw=[tp])
            kb.op("dve", lambda e: e.tensor_scalar(blast[:], bneg[:, :, 63], -1.0, None, ALU.mult), rw=[tp])
            kb.op("pe", lambda e: e.transpose(psA[0:4, 0:NCH], amax[:], ident_f[0:NCH, 0:NCH]), reads=[tp, t_const], writes=[t_psA])
            kb.op("dve", lambda e: e.tensor_copy(amaxT[:], psA[0:4, 0:NCH]), reads=[t_psA], rw=[tp])
            kb.op("pe", lambda e: e.transpose(psA[0:4, 0:NCH], blast[:], ident_f[0:NCH, 0:NCH]), reads=[tp, t_const], writes=[t_psA])
            kb.op("dve", lambda e: e.tensor_copy(blastT[:], psA[0:4, 0:NCH]), reads=[t_psA], rw=[tp])
            kb.op("dve", lambda e: e.tensor_tensor_scan(mrow[:], amaxT[:], blastT[:], 0.0, ALU.max, ALU.add), rw=[tp])
            kb.op("dve", lambda e: e.tensor_tensor(Mend[:], mrow[:], blastT[:], ALU.subtract), rw=[tp])
            kb.op("dve", lambda e: e.memset(mprev[:, 0:1], 0.0), rw=[tp])
            if NCH > 1:
                kb.op("dve", lambda e: e.tensor_copy(mprev[:, 1:NCH], mrow[:, 0:NCH - 1]), rw=[tp])
            kb.op("dve", lambda e: e.tensor_tensor(wold[:], mprev[:], Mend[:], ALU.subtract), rw=[tp])
            kb.op("act", lambda e: e.activation(wold[:], wold[:], AF.Exp), rw=[tp])
            kb.op("pe", lambda e: e.transpose(psA[0:NCH, 0:4], Mend[:], ident_f[0:4, 0:4]), reads=[tp, t_const], writes=[t_psA])
            kb.op("dve", lambda e: e.tensor_scalar(negM[:], psA[0:NCH, 0:4], -1.0, None, ALU.mult), reads=[t_psA], rw=[tp])
            for h in range(4):
                kb.op("act", lambda e, h=h: e.activation(u_t[:, h, :], a_t[:, h, :], AF.Exp, bias=negM[:, h:h + 1]), rw=[tp])
                kb.op("act", lambda e, h=h: e.activation(fl_t[:, h, :], bneg[:, h, :], AF.Exp, bias=negM[:, h:h + 1]), rw=[tp])
            for (src_t, dst_t) in ((u_t, uT), (fl_t, flT)):
                for h in range(4):
                    kb.op("pe", lambda e, h=h, src_t=src_t: e.transpose(psA[0:64, 0:NCH], src_t[:, h, :], ident_f[0:NCH, 0:NCH]),
                          reads=[tp, t_const], writes=[t_psA])
                    kb.op("dve", lambda e, h=h, dst_t=dst_t: e.tensor_copy(dst_t[:, h, :], psA[0:64, 0:NCH]), reads=[t_psA], rw=[tp])
            for h in range(4):
                kb.op("pe", lambda e, h=h: e.matmul(psA[:, 0:NCH], sel4s[:, h, :], wold[:], start=True, stop=True),
                      reads=[tp, tg], writes=[t_psA])
                kb.op("dve", lambda e, h=h: e.tensor_copy(wb[:, h, :], psA[:, 0:NCH]), reads=[t_psA], rw=[tp])
            t_qk, t_vt, t_so = [T(), T()], [T(), T()], [T(), T()]
            t_C32 = [T(), T()]
            t_Csb, t_pk, t_pp, t_pacc, t_pc = T(), T(), T(), T(), T()
            t_ktok2, t_vp2, t_scT2 = [T(), T()], [T(), T()], [T(), T()]
            t_den2, t_hb_2, t_hsq2, t_ssq22, t_hb22 = [T(), T()], [T(), T()], [T(), T()], [T(), T()], [T(), T()]
            t_os = [T(), T()]
            t_gso, t_accS, t_sa, t_tq, t_junkS = [T(), T()], [T(), T()], [T(), T()], [T(), T()], T()
            for i_ in range(2):
                kb.op("pool", lambda e, i_=i_: e.memset(vt[i_][:, :, :, 128:129], 1.0), writes=[t_vt[i_]])
            kb.op("dve", lambda e: e.memset(C32[1][:], 0.0), writes=[t_C32[1]])
            FMv2 = FM.rearrange("g p t -> p g t")
            TMc = TM.rearrange("(n cc l) c -> n l cc c", cc=8, l=64)
            HCc = HC.rearrange("(n cc l) c -> n l cc c", cc=8, l=64)

            def loadB(blk):
                bf = blk % 2
                kb.dma("sp", qk[bf][:], FMv2[:, 0:8, blk * 512:(blk + 1) * 512], t_qk[bf], src=t_FM)
                for h_ in range(4):
                    kb.dma("sp", vt[bf][:, :, h_, 0:128], TMc[blk][:, :, h_ * 128:(h_ + 1) * 128], t_vt[bf], src=t_TM)
                kb.dma("sp", so[bf][:], TMc[blk][:, :, 512:1024], t_so[bf], src=t_TM)
                kb.op("pool", lambda e, bf=bf: e.tensor_tensor(gso[bf][:], so[bf][:], gmB[:].unsqueeze(1).to_broadcast([64, 8, 512]), ALU.mult),
                      reads=[t_so[bf], tg], writes=[t_gso[bf]])

            def outchain(c):
                blk_, cc_ = divmod(c, 8)
                bf_, cur_ = blk_ % 2, c % 2
                aS, den, tq, hb2, sa = accS[cur_], den_2[cur_], tq_2[cur_], hb2_2[cur_], sa_2[cur_]
                t_aS, t_dn, t_tq_, t_h2, t_sa_ = t_accS[cur_], t_den2[cur_], t_tq[cur_], t_hb22[cur_], t_sa[cur_]
                for h in range(4):
                    kb.op("act", lambda e, h=h, aS=aS, sa=sa: e.activation(junkS[:], aS[:, h, 0:128], AF.Square, accum_out=sa[:, h, :]),
                          reads=[t_aS], writes=[t_junkS], pw=[t_sa_])
                kb.op("dve", lambda e, aS=aS, den=den, c=c: e.tensor_tensor(den[:], aS[:, :, 128:129], flT[:, :, c:c + 1], ALU.max),
                      reads=[t_aS, tp], writes=[t_dn])
                kb.op("dve", lambda e, aS=aS, den=den: e.scalar_tensor_tensor(den[:], aS[:, :, 128:129], -1.0, den[:], ALU.mult, ALU.max),
                      reads=[t_aS], rw=[t_dn])
                kb.op("dve", lambda e, den=den: e.reciprocal(den[:], den[:]), rw=[t_dn])
                kb.op("dve", lambda e, tq=tq, sa=sa, den=den: e.tensor_tensor(tq[:], sa[:], den[:], ALU.mult), reads=[t_sa_, t_dn], writes=[t_tq_])
                kb.op("dve", lambda e, tq=tq, den=den: e.tensor_tensor(tq[:], tq[:], den[:], ALU.mult), reads=[t_dn], rw=[t_tq_])
                kb.op("act", lambda e, tq=tq: e.activation(tq[:], tq[:], AF.Sqrt, scale=1.0 / 128, bias=EPS), rw=[t_tq_])
                kb.op("dve", lambda e, tq=tq: e.reciprocal(tq[:], tq[:]), rw=[t_tq_])
                kb.op("dve", lambda e, tq=tq, den=den: e.tensor_tensor(tq[:], tq[:], den[:], ALU.mult), reads=[t_dn], rw=[t_tq_])
                kb.op("dve", lambda e, hb2=hb2, aS=aS, tq=tq: e.tensor_tensor(hb2[:], aS[:, :, 0:128], tq[:].to_broadcast([64, 4, 128]), ALU.mult),
                      reads=[t_aS, t_tq_], writes=[t_h2])
                kb.op("pool", lambda e, hb2=hb2, bf_=bf_, cc_=cc_: e.tensor_tensor(ostage[bf_][:, cc_, :].rearrange("p (h e) -> p h e", h=4), hb2[:],
                                                                              gso[bf_][:, cc_, :].rearrange("p (h e) -> p h e", h=4), ALU.mult),
                      reads=[t_h2, t_gso[bf_]], pw=[t_os[bf_]])
                if cc_ == 7:
                    kb.dma("sp", HCc[blk_][:, :, 0:512], ostage[bf_][:], t_HC, src=t_os[bf_], store=True)

            loadB(0)
            for blk in range(NT):
                bf = blk % 2
                for cc in range(8):
                    c = blk * 8 + cc
                    cur, prv = c % 2, 1 - (c % 2)
                    ktok, vp, scT, den, hb, hsq, ssq2, hb2 = ktok_2[cur], vp_2[cur], scT_2[cur], den_2[cur], hb_2[cur], hsq_2[cur], ssq2_2[cur], hb2_2[cur]
                    t_ktok, t_vp, t_scT, t_den, t_hb, t_hsq, t_ssq2, t_hb2 = t_ktok2[cur], t_vp2[cur], t_scT2[cur], t_den2[cur], t_hb_2[cur], t_hsq2[cur], t_ssq22[cur], t_hb22[cur]
                    sl = slice(cc * 64, (cc + 1) * 64)
                    for h in range(4):
                        kb.op("act", lambda e, ktok=ktok, vp=vp, scT=scT, den=den, hb=hb, hsq=hsq, ssq2=ssq2, hb2=hb2, h=h, prv=prv, c=c: e.activation(Csb[:, h, :], C32[prv][:, h, :], AF.Identity,
                                                                              scale=wb[:, h, c:c + 1]),
                              reads=[t_C32[prv], tp], pw=[t_Csb])
                    for h in range(4):
                        kb.op("pe", lambda e, ktok=ktok, vp=vp, scT=scT, den=den, hb=hb, hsq=hsq, ssq2=ssq2, hb2=hb2, h=h, bf=bf, sl=sl: e.transpose(pkv[:, h, :], qk[bf][:, 4 + h, sl], ident_b[:]),
                              reads=[t_qk[bf], t_const], writes=[t_pk])
                    kb.op("act", lambda e, ktok=ktok, vp=vp, scT=scT, den=den, hb=hb, hsq=hsq, ssq2=ssq2, hb2=hb2: e.activation(ktok[:], pkv, AF.Identity), reads=[t_pk], writes=[t_ktok])
                    kb.op("dve", lambda e, vp=vp, bf=bf, cc=cc, c=c: e.tensor_tensor(vp[:, :, 0:129], vt[bf][:, cc, :, :],
                                                                          uT[:, :, c:c + 1].to_broadcast([64, 4, 129]), ALU.mult),
                          reads=[t_vt[bf], tp], writes=[t_vp])
                    for h in range(4):
                        kb.op("pe", lambda e, ktok=ktok, vp=vp, scT=scT, den=den, hb=hb, hsq=hsq, ssq2=ssq2, hb2=hb2, h=h, bf=bf, sl=sl: e.matmul(ppv[:, h, :], qk[bf][:, 4 + h, sl], qk[bf][:, h, sl], start=True, stop=True),
                              reads=[t_qk[bf]], writes=[t_pp])
                    kb.op("dve", lambda e, ktok=ktok, vp=vp, scT=scT, den=den, hb=hb, hsq=hsq, ssq2=ssq2, hb2=hb2: e.tensor_tensor(scT[:], ppv, mask64[:].unsqueeze(1).to_broadcast([64, 4, 64]), ALU.mult),
                          reads=[t_pp, tg], writes=[t_scT])
                    for h in range(4):
                        kb.op("pe", lambda e, ktok=ktok, vp=vp, scT=scT, den=den, hb=hb, hsq=hsq, ssq2=ssq2, hb2=hb2, h=h, bf=bf, sl=sl: e.matmul(paccv[:, h, 0:129], qk[bf][:, h, sl], Csb[:, h, :], start=True, stop=False),
                              reads=[t_qk[bf], t_Csb], writes=[t_pacc])
                        kb.op("pe", lambda e, ktok=ktok, vp=vp, scT=scT, den=den, hb=hb, hsq=hsq, ssq2=ssq2, hb2=hb2, h=h: e.matmul(paccv[:, h, 0:129], scT[:, h, :], vp[:, h, 0:129], start=False, stop=True),
                              reads=[t_scT, t_vp], writes=[t_pacc])
                    for h in range(4):
                        kb.op("pe", lambda e, ktok=ktok, vp=vp, scT=scT, den=den, hb=hb, hsq=hsq, ssq2=ssq2, hb2=hb2, h=h: e.matmul(pcv[:, h, 0:129], ktok[:, h, :], vp[:, h, 0:129], start=True, stop=True),
                              reads=[t_ktok, t_vp], writes=[t_pc])
                    for h in range(4):
                        kb.op("dve", lambda e, ktok=ktok, vp=vp, scT=scT, den=den, hb=hb, hsq=hsq, ssq2=ssq2, hb2=hb2, h=h, cur=cur, prv=prv, c=c: e.scalar_tensor_tensor(C32[cur][:, h, :], C32[prv][:, h, :], wb[:, h, c:c + 1],
                                                                                               pcv[:, h, 0:129], ALU.mult, ALU.add),
                              reads=[t_C32[prv], t_pc, tp], pw=[t_C32[cur]])
                    kb.op("act", lambda e, cur=cur: e.activation(accS[cur][:], paccv[:, :, 0:129], AF.Identity), reads=[t_pacc], writes=[t_accS[cur]])
                    if c > 0:
                        outchain(c - 1)
                    if cc == 0 and blk + 1 < NT:
                        loadB(blk + 1)
            outchain(NCH - 1)
            kb.barrier()

        if stop <= 2:
            return nc
        with ExitStack() as ph:
            KT = [sbuf(ph, "KT%d" % i, [128, S], BF16) for i in range(2)]
            VA = [sbuf(ph, "VA%d" % i, [128, NS, 65], BF16) for i in range(2)]
            QT = [sbuf(ph, "QT%d" % i, [128, 512], BF16) for i in range(2)]
            kms = sbuf(ph, "kms", [64, NB], F32)
            kmh = sbuf(ph, "kmh", [128, NB], BF16)
            kml = sbuf(ph, "kml", [128, NB], BF16)
            kmd = sbuf(ph, "kmd", [64, NB], F32)
            gsb = [sbuf(ph, "gsb%d" % i, [128, max(NB, 8)], F32) for i in range(4)]
            m8_4 = sbuf(ph, "m8_4", [128, 4, 8], F32)
            gsb4 = sbuf(ph, "gsb4", [128, 4, max(NB, 8)], F32)
            t_gsb4 = T()
            selq = [sbuf(ph, "selq%d" % i, [128, 4, max(NB, 8)], F32) for i in range(2)]
            tmpA = [sbuf(ph, "tmpA%d" % i, [128, 4, 65], F32) for i in range(2)]
            PT = [sbuf(ph, "PT%d" % i, [128, 512], BF16) for i in range(4)]
            accA = [sbuf(ph, "accA%d" % i, [128, 4, 65], F32) for i in range(2)]
            rdn = sbuf(ph, "rdn", [128, 4, 1], F32)
            oa = [sbuf(ph, "oa%d" % i, [128, 4, 64], BF16) for i in range(2)]
            pS = [psum(ph, "pS%d" % i, [128, 512], F32) for i in range(4)]
            pO = [psum(ph, "pO%d" % i, [128, 512], F32) for i in range(2)]
            pG = psum(ph, "pG", [128, 512], F32)
            t_KT, t_VA, t_QT = [T(), T()], [T(), T()], [T(), T()]
            t_km, t_gsb, t_m8, t_sel = T(), [T() for _ in range(4)], T(), [T(), T()]
            t_tmpA = [T(), T()]
            cnt_t = [0]
            t_PT, t_pS, t_pO = [T() for _ in range(4)], [T() for _ in range(4)], [T(), T()]
            t_pG, t_acc, t_rdn, t_oa = T(), [T(), T()], T(), [T(), T()]
            FMp = FM
            TMn = TM.rearrange("(n p) c -> p n c", p=128)
            HCq = HC.rearrange("(n s p) c -> n p s c", s=4, p=128)
            for i in range(2):
                kb.op("pool", lambda e, i=i: e.memset(VA[i][:, :, 64:65], 1.0), writes=[t_VA[i]])
                kb.op("pool", lambda e, i=i: e.memset(KT[i][64:128, :], 0.0), writes=[t_KT[i]])
                kb.op("pool", lambda e, i=i: e.memset(QT[i][64:128, :], 0.0), writes=[t_QT[i]])
            kb.op("pool", lambda e: e.memset(kmh[:], 0.0), writes=[t_km])
            kb.op("pool", lambda e: e.memset(kml[:], 0.0), rw=[t_km])

            def loadH(h):
                bf = h % 2
                kb.dma("sp", KT[bf][0:64, :], FMp[12 + h // 2, (h % 2) * 64:(h % 2) * 64 + 64, :], t_KT[bf], src=t_FM)
                kb.dma("sp", VA[bf][:, :, 0:64], TMn[:, :, 1024 + h * 64:1024 + (h + 1) * 64], t_VA[bf], src=t_TM)

            cnt_s, cnt_o, cnt_p, cnt_q = [0], [0], [0], [0]
            loadH(0)
            for h in range(8):
                hb_ = h % 2
                if h + 1 < 8:
                    loadH(h + 1)
                Kc, Vc = KT[hb_], VA[hb_]
                kb.op("dve", lambda e, Kc=Kc: e.tensor_reduce(kms[:], Kc[0:64, :].rearrange("p (n k) -> p n k", k=256), AX.X, ALU.add),
                      reads=[t_KT[hb_]], writes=[t_km])
                kb.op("dve", lambda e: e.tensor_copy(kmh[0:64, :], kms[:]), rw=[t_km])
                kb.op("dve", lambda e: e.tensor_tensor(kmd[:], kms[:], kmh[0:64, :], ALU.subtract), rw=[t_km])
                kb.op("dve", lambda e: e.tensor_copy(kml[0:64, :], kmd[:]), rw=[t_km])
                units = []
                for i in range(NT):
                    for qs in range(4):
                        for kh in ([0] if qs % 2 == 0 else [0, 1]):
                            units.append(("own", i, qs, kh))
                    for b in range(2 * i + 1):
                        for kh in range(2):
                            units.append(("past", i, b, kh))

                def loadQ(i):
                    kb.dma("sp", QT[i % 2][0:64, :], FMp[8 + h // 2, (h % 2) * 64:(h % 2) * 64 + 64, i * 512:(i + 1) * 512], t_QT[i % 2], src=t_FM)

                def gating(i):
                    qb = i % 2
                    Qc = QT[qb]
                    sq, ts_ = selq[i % 2], t_sel[i % 2]
                    pGv = pG[:, 0:4 * 32].rearrange("p (q n) -> p q n", q=4)
                    big = [qs for qs in range(4) if 2 * i + qs // 2 > 3]
                    for qs in range(4):
                        own = 2 * i + qs // 2
                        if 0 < own <= 3:
                            kb.op("dve", lambda e, qs=qs, sq=sq: e.memset(sq[:, qs, :], 1.0), pw=[ts_])
                    if not big:
                        return
                    for qs in big:
                        kb.op("pe", lambda e, qs=qs, Qc=Qc: e.matmul(pG[:, qs * 32:qs * 32 + NB], Qc[:, qs * 128:(qs + 1) * 128], kmh[:], start=True, stop=False),
                              reads=[t_QT[qb], t_km], writes=[t_pG])
                        kb.op("pe", lambda e, qs=qs, Qc=Qc: e.matmul(pG[:, qs * 32:qs * 32 + NB], Qc[:, qs * 128:(qs + 1) * 128], kml[:], start=False, stop=True),
                              reads=[t_QT[qb], t_km], writes=[t_pG])
                    kb.op("dve", lambda e: e.memset(gsb4[:], -BIG), writes=[t_gsb4])
                    for p in range(2):
                        own = 2 * i + p
                        if own > 3:
                            kb.op("dve", lambda e, p=p, own=own: e.tensor_copy(gsb4[:, 2 * p:2 * p + 2, 0:own], pGv[:, 2 * p:2 * p + 2, 0:own]),
                                  reads=[t_pG], pw=[t_gsb4])
                    for qs in big:
                        kb.op("dve", lambda e, qs=qs: e.max(m8_4[:, qs, :], gsb4[:, qs, :]), reads=[t_gsb4], pw=[t_m8])
                    for p in range(2):
                        own = 2 * i + p
                        if own > 3:
                            kb.op("dve", lambda e, p=p, own=own, sq=sq: e.tensor_tensor(sq[:, 2 * p:2 * p + 2, 0:own], gsb4[:, 2 * p:2 * p + 2, 0:own],
                                                                                     m8_4[:, 2 * p:2 * p + 2, 2:3].to_broadcast([128, 2, own]), ALU.is_ge),
                                  reads=[t_gsb4, t_m8], pw=[ts_])

                def stageA(u):
                    kind, i, x_, kh = u
                    qb = i % 2
                    Qc = QT[qb]
                    sb_ = cnt_s[0] % 4
                    cnt_s[0] += 1
                    if kind == "own":
                        qs = x_
                        own = 2 * i + qs // 2
                        k0 = own * 256 + kh * 128
                        kb.op("pe", lambda e, sb_=sb_, k0=k0, qs=qs, Qc=Qc: e.matmul(pS[sb_][:, 0:128], Kc[:, k0:k0 + 128], Qc[:, qs * 128:(qs + 1) * 128],
                                                                                  start=True, stop=True),
                              reads=[t_KT[hb_], t_QT[qb]], writes=[t_pS[sb_]])
                        kb.op("act", lambda e, sb_=sb_: e.activation(PT[sb_][:, 0:128], pS[sb_][:, 0:128], AF.Exp),
                              reads=[t_pS[sb_]], writes=[t_PT[sb_]])
                        if kh == qs % 2:
                            kb.op("pool", lambda e, sb_=sb_: e.tensor_tensor(PT[sb_][:, 0:128], PT[sb_][:, 0:128], tri_b[:], ALU.mult),
                                  reads=[t_const], rw=[t_PT[sb_]])
                    else:
                        b = x_
                        q0 = 0 if b < 2 * i else 2
                        k0 = b * 256 + kh * 128
                        kb.op("pe", lambda e, sb_=sb_, k0=k0, q0=q0, Qc=Qc: e.matmul(pS[sb_][:, q0 * 128:512], Kc[:, k0:k0 + 128], Qc[:, q0 * 128:512],
                                                                                  start=True, stop=True),
                              reads=[t_KT[hb_], t_QT[qb]], writes=[t_pS[sb_]])
                        kb.op("act", lambda e, sb_=sb_, q0=q0: e.activation(PT[sb_][:, q0 * 128:512], pS[sb_][:, q0 * 128:512], AF.Exp),
                              reads=[t_pS[sb_]], writes=[t_PT[sb_]])
                    return sb_

                def stageB(u, sb_):
                    kind, i, x_, kh = u
                    ac, t_ac = accA[i % 2], t_acc[i % 2]
                    if kh == 0:
                        cnt_o[0] += 1
                    ob = cnt_o[0] % 2
                    if kind == "own":
                        qs = x_
                        own = 2 * i + qs // 2
                        last = 0 if qs % 2 == 0 else 1
                        kb.op("pe", lambda e, ob=ob, sb_=sb_, kh=kh, own=own, last=last: e.matmul(pO[ob][:, 0:65], PT[sb_][:, 0:128], Vc[:, own * 2 + kh, :],
                                                                                              start=(kh == 0), stop=(kh == last)),
                              reads=[t_PT[sb_], t_VA[hb_]], writes=[t_pO[ob]])
                        if kh == last:
                            kb.op("dve", lambda e, ob=ob, qs=qs, ac=ac: e.tensor_copy(ac[:, qs, :], pO[ob][:, 0:65]), reads=[t_pO[ob]], pw=[t_ac])
                    else:
                        b = x_
                        q0 = 0 if b < 2 * i else 2
                        for qs in range(q0, 4):
                            kb.op("pe", lambda e, ob=ob, sb_=sb_, kh=kh, qs=qs, b=b, q0=q0: e.matmul(pO[ob][:, qs * 128:qs * 128 + 65], PT[sb_][:, qs * 128:(qs + 1) * 128],
                                                                                                  Vc[:, b * 2 + kh, :], start=(kh == 0 and qs == q0), stop=(kh == 1),
                                                                                                  skip_group_check=True),
                                  reads=[t_PT[sb_], t_VA[hb_]], writes=[t_pO[ob]])
                        if kh == 1:
                            tb = cnt_t[0] % 2
                            cnt_t[0] += 1
                            pov = pO[ob][:].rearrange("p (q e) -> p q e", q=4)
                            nq = 4 - q0
                            kb.op("dve", lambda e, tb=tb, pov=pov, q0=q0, nq=nq, b=b, i=i: e.tensor_tensor(
                                tmpA[tb][:, q0:4, :], pov[:, q0:4, 0:65], selq[i % 2][:, q0:4, b:b + 1].to_broadcast([128, nq, 65]), ALU.mult),
                                reads=[t_pO[ob], t_sel[i % 2]], writes=[t_tmpA[tb]])
                            kb.op("dve", lambda e, tb=tb, q0=q0, ac=ac: e.tensor_tensor(ac[:, q0:4, :], ac[:, q0:4, :], tmpA[tb][:, q0:4, :], ALU.add),
                                  reads=[t_tmpA[tb]], rw=[t_ac])

                def finalize(i):
                    ac, t_ac = accA[i % 2], t_acc[i % 2]
                    ab = i % 2
                    kb.op("dve", lambda e, ac=ac: e.reciprocal(rdn[:], ac[:, :, 64:65]), reads=[t_ac], writes=[t_rdn])
                    kb.op("dve", lambda e, ab=ab, ac=ac: e.tensor_tensor(oa[ab][:], ac[:, :, 0:64], rdn[:].to_broadcast([128, 4, 64]), ALU.mult),
                          reads=[t_ac, t_rdn], writes=[t_oa[ab]])
                    kb.dma("sp", HCq[i][:, :, 512 + h * 64:512 + (h + 1) * 64], oa[ab][:], t_HC, src=t_oa[ab], store=True)

                def enter_tile(i):
                    if i + 1 < NT:
                        loadQ(i + 1)
                    gating(i)

                loadQ(0)
                LA = 3
                entered = set()

                def issueA(n):
                    ti_ = units[n][1]
                    if ti_ not in entered:
                        entered.add(ti_)
                        enter_tile(ti_)
                    return stageA(units[n])

                sbs = {}
                for n in range(min(LA, len(units))):
                    sbs[n] = issueA(n)
                for n, u in enumerate(units):
                    if n + LA < len(units):
                        sbs[n + LA] = issueA(n + LA)
                    stageB(u, sbs.pop(n))
                    if n + 1 == len(units) or units[n + 1][1] != u[1]:
                        finalize(u[1])
            kb.barrier()

        if stop <= 3:
            return nc
        with ExitStack() as ph:
            w_out_sb = sbuf(ph, "w_out_sb", [128, 8, D], BF16)
            w_r_sb = sbuf(ph, "w_r_sb", [128, 8, 36], BF16)
            brB = sbuf(ph, "brB", [128, 36], F32)
            hct = [sbuf(ph, "hct%d" % i, [128, 4, D], BF16) for i in range(2)]
            xt = [sbuf(ph, "xtD%d" % i, [128, 4, D], F32) for i in range(2)]
            hcT = sbuf(ph, "hcT", [128, 8, 512], BF16)
            x1 = sbuf(ph, "x1", [128, 4, D], F32)
            tmpD2 = [sbuf(ph, "tmpD%d" % i, [128, 512], F32) for i in range(2)]
            junk = sbuf(ph, "junkD", [128, D], BF16)
            ssq = sbuf(ph, "ssqD", [128, 4], F32)
            rs = sbuf(ph, "rsD", [128, 4], F32)
            xn = sbuf(ph, "xnD", [128, 4, D], BF16)
            hn2T = [sbuf(ph, "hn2T%d" % i, [128, 8, 512], BF16) for i in range(2)]
            lg = sbuf(ph, "lg", [128, 36], F32)
            lmax = sbuf(ph, "lmax", [128, 1], F32)
            nlmax = sbuf(ph, "nlmax", [128, 1], F32)
            eg = sbuf(ph, "eg", [128, 4], F32)
            gsum = sbuf(ph, "gsum", [128, 1], F32)
            ohg = sbuf(ph, "ohg", [128, 4], F32)
            me = sbuf(ph, "me", [128, 32], F32)
            m8r = sbuf(ph, "m8r", [128, 8], F32)
            e2 = sbuf(ph, "e2", [128, 1], F32)
            w1 = sbuf(ph, "w1", [128, 1], F32)
            w2 = sbuf(ph, "w2", [128, 1], F32)
            t1 = sbuf(ph, "t1", [128, 32], F32)
            t2 = sbuf(ph, "t2", [128, 32], F32)
            pT = [psum(ph, "pTD%d" % i, [128, 1024], BF16) for i in range(2)]
            pm = [psum(ph, "pmD%d" % i, [128, 512], F32) for i in range(4)]
            pr = psum(ph, "prD", [128, 512], F32)
            t_w, t_hct, t_xt = T(), [T(), T()], [T(), T()]
            t_hcT, t_x1, t_tmp2, t_junk, t_ssq, t_rs, t_xn = T(), T(), [T(), T()], T(), T(), T(), T()
            t_hn2T, t_pT, t_pm, t_pr, t_r = [T(), T()], [T(), T()], [T() for _ in range(4)], T(), T()
            kb.dma("pool", w_out_sb[:], w_out.rearrange("(kc p) n -> p kc n", p=128), t_w)
            kb.dma("pool", w_r_sb[:], w_r.rearrange("(kc p) n -> p kc n", p=128), t_w)
            kb.dma("sp", brB[:], b_r.to_broadcast([128, 36]), t_w)
            xv = x.rearrange("(n s p) d -> n p s d", s=4, p=128)
            HCv = HC.rearrange("(n s p) d -> n p s d", s=4, p=128)
            X1v = X1.rearrange("(n s p) d -> n p s d", s=4, p=128)
            XNv = XN.rearrange("(n s p) d -> n p s d", s=4, p=128)
            kb.dma("sp", hct[0][:], HCv[0], t_hct[0], src=t_HC)
            kb.dma("sp", xt[0][:], xv[0], t_xt[0])
            def router(i):
                cur = i % 2
                hc2 = hn2T[cur]
                for s in range(4):
                    ti = i * 4 + s
                    for kc in range(8):
                        kb.op("pe", lambda e, kc=kc, s=s: e.matmul(pr[:, 0:36], hc2[:, kc, s * 128:(s + 1) * 128], w_r_sb[:, kc, :], start=(kc == 0), stop=(kc == 7)),
                              reads=[t_hn2T[cur], t_w], writes=[t_pr])
                    kb.op("dve", lambda e: e.tensor_tensor(lg[:], pr[:, 0:36], brB[:], ALU.add), reads=[t_pr, t_w], writes=[t_r])
                    kb.op("dve", lambda e: e.tensor_reduce(lmax[:], lg[:, 0:4], AX.X, ALU.max), rw=[t_r])
                    kb.op("dve", lambda e: e.tensor_scalar(nlmax[:], lmax[:], -1.0, None, ALU.mult), rw=[t_r])
                    kb.op("act", lambda e: e.activation(eg[:], lg[:, 0:4], AF.Exp, bias=nlmax[:, 0:1], accum_out=gsum[:]), rw=[t_r])
                    kb.op("dve", lambda e: e.reciprocal(gsum[:], gsum[:]), rw=[t_r])
                    kb.op("dve", lambda e: e.tensor_scalar(ohg[:], lg[:, 0:4], lmax[:, 0:1], None, ALU.is_ge), rw=[t_r])
                    kb.op("dve", lambda e: e.tensor_scalar(ohg[:], ohg[:], BIG, -BIG, ALU.mult, ALU.add), rw=[t_r])
                    kb.op("dve", lambda e: e.tensor_tensor(me[:].rearrange("p (g k) -> p g k", g=4), lg[:, 4:36].rearrange("p (g k) -> p g k", g=4),
                                                           ohg[:].unsqueeze(2).to_broadcast([128, 4, 8]), ALU.add), rw=[t_r])
                    kb.op("dve", lambda e: e.max(m8r[:], me[:]), rw=[t_r])
                    kb.op("dve", lambda e: e.tensor_tensor(e2[:], m8r[:, 1:2], m8r[:, 0:1], ALU.subtract), rw=[t_r])
                    kb.op("act", lambda e: e.activation(e2[:], e2[:], AF.Exp), rw=[t_r])
                    kb.op("dve", lambda e: e.tensor_scalar(w1[:], e2[:], 1.0, None, ALU.add), rw=[t_r])
                    kb.op("dve", lambda e: e.reciprocal(w1[:], w1[:]), rw=[t_r])
                    kb.op("dve", lambda e: e.tensor_tensor(w1[:], w1[:], gsum[:], ALU.mult), rw=[t_r])
                    kb.op("dve", lambda e: e.tensor_tensor(w2[:], w1[:], e2[:], ALU.mult), rw=[t_r])
                    kb.op("dve", lambda e, ti=ti: e.tensor_scalar(OH1a[:, ti, :], me[:], m8r[:, 0:1], None, ALU.is_equal), reads=[t_r], pw=[t_rt])
                    kb.op("dve", lambda e, ti=ti: e.tensor_scalar(OH2a[:, ti, :], me[:], m8r[:, 1:2], None, ALU.is_equal), reads=[t_r], pw=[t_rt])
                    kb.op("dve", lambda e, ti=ti: e.tensor_copy(w1a[:, ti:ti + 1], w1[:]), reads=[t_r], pw=[t_rt])
                    kb.op("dve", lambda e, ti=ti: e.tensor_copy(w2a[:, ti:ti + 1], w2[:]), reads=[t_r], pw=[t_rt])

            pmi = [0]
            for i in range(NT):
                cur = i % 2
                if i + 1 < NT:
                    kb.dma("sp", hct[1 - cur][:], HCv[i + 1], t_hct[1 - cur], src=t_HC)
                    kb.dma("sp", xt[1 - cur][:], xv[i + 1], t_xt[1 - cur])
                for kc in range(8):
                    pb = kc % 2
                    for s in range(4):
                        kb.op("pe", lambda e, kc=kc, s=s, pb=pb, cur=cur: e.transpose(pT[pb][:, s * 128:(s + 1) * 128], hct[cur][:, s, kc * 128:(kc + 1) * 128], ident_b[:]),
                              reads=[t_hct[cur], t_const], writes=[t_pT[pb]])
                    kb.op("act", lambda e, kc=kc, pb=pb: e.activation(hcT[:, kc, :], pT[pb][:, 0:512], AF.Identity), reads=[t_pT[pb]], pw=[t_hcT])
                for s in range(4):
                    for hf in range(2):
                        pi = pmi[0] % 4
                        pmi[0] += 1
                        for kc in range(8):
                            kb.op("pe", lambda e, kc=kc, s=s, hf=hf, pi=pi: e.matmul(pm[pi][:, :], hcT[:, kc, s * 128:(s + 1) * 128], w_out_sb[:, kc, hf * 512:(hf + 1) * 512],
                                                                                   start=(kc == 0), stop=(kc == 7)),
                                  reads=[t_hcT, t_w], writes=[t_pm[pi]])
                        tmpD, t_tmp = tmpD2[pi % 2], t_tmp2[pi % 2]
                        kb.op("dve", lambda e, hf=hf, pi=pi, tmpD=tmpD: e.tensor_tensor(tmpD[:], pm[pi][:, :], gate1B[:, hf * 512:(hf + 1) * 512], ALU.mult),
                              reads=[t_pm[pi], t_mod], writes=[t_tmp])
                        kb.op("pool", lambda e, s=s, hf=hf, cur=cur, tmpD=tmpD: e.tensor_tensor(x1[:, s, hf * 512:(hf + 1) * 512], tmpD[:], xt[cur][:, s, hf * 512:(hf + 1) * 512], ALU.add),
                              reads=[t_tmp, t_xt[cur]], pw=[t_x1])
                kb.dma("sp", X1v[i], x1[:], t_X1, src=t_x1, store=True)
                if i > 0:
                    router(i - 1)
                for s in range(4):
                    kb.op("act", lambda e, s=s: e.activation(junk[:], x1[:, s, :], AF.Square, accum_out=ssq[:, s:s + 1]), reads=[t_x1], writes=[t_junk, t_ssq])
                kb.op("act", lambda e: e.activation(rs[:], ssq[:], AF.Sqrt, scale=1.0 / D, bias=EPS), reads=[t_ssq], writes=[t_rs])
                kb.op("dve", lambda e: e.reciprocal(rs[:], rs[:]), rw=[t_rs])
                for s in range(4):
                    kb.op("dve", lambda e, s=s: e.tensor_scalar(xn[:, s, :], x1[:, s, :], rs[:, s:s + 1], None, ALU.mult), reads=[t_x1, t_rs], pw=[t_xn])
                hc2 = hn2T[cur]
                for kc in range(8):
                    pb = kc % 2
                    for s in range(4):
                        kb.op("pe", lambda e, kc=kc, s=s, pb=pb: e.transpose(pT[pb][:, s * 128:(s + 1) * 128], xn[:, s, kc * 128:(kc + 1) * 128], ident_b[:]),
                              reads=[t_xn, t_const], writes=[t_pT[pb]])
                    if kc % 2 == 0:
                        kb.op("act", lambda e, kc=kc, pb=pb: e.activation(hc2[:, kc, :], pT[pb][:, 0:512], AF.Identity, scale=a2[:, kc:kc + 1], bias=b2[:, kc:kc + 1]),
                              reads=[t_pT[pb], t_mod], pw=[t_hn2T[cur]])
                    else:
                        kb.op("dve", lambda e, kc=kc, pb=pb: e.tensor_scalar(hc2[:, kc, :], pT[pb][:, 0:512], a2[:, kc:kc + 1], b2[:, kc:kc + 1], ALU.mult, ALU.add),
                              reads=[t_pT[pb], t_mod], pw=[t_hn2T[cur]])
                kb.dma("sp", XNv[i], xn[:], t_XN, src=t_xn, store=True)
            router(NT - 1)
            kb.barrier()

        if stop <= 4:
            return nc
        with ExitStack() as ph:
            OHs = sbuf(ph, "OHs", [128, NS, NEXP], F32)
            OHsb = sbuf(ph, "OHsb", [128, NS, NEXP], BF16)
            Cum = sbuf(ph, "Cum", [128, NS, NEXP], F32)
            prod = sbuf(ph, "prod", [128, NS, NEXP], F32)
            R = sbuf(ph, "R", [128, NEXP], F32)
            trs_b = sbuf(ph, "trs_b", [128, 128], BF16)
            ones_b = sbuf(ph, "ones_b", [128, 128], BF16)
            thr = sbuf(ph, "thr_s", [128, NBLK], F32)
            hp = sbuf(ph, "hp_s", [128, 2], F32)
            rank1 = sbuf(ph, "rank1", [128, NS], F32)
            rank2 = sbuf(ph, "rank2", [128, NS], F32)
            base = sbuf(ph, "base", [128, NS], F32)
            ci = sbuf(ph, "ci", [128, NEXP], I32)
            padf = sbuf(ph, "padf", [128, NEXP], F32)
            pend = sbuf(ph, "pend", [128, NEXP], F32)
            pstart = sbuf(ph, "pstart", [128, NEXP], F32)
            T3 = sbuf(ph, "T3", [128, NBLK, NEXP], F32)
            be = sbuf(ph, "be", [128, NBLK], F32)
            psC = psum(ph, "psC", [128, 512], F32)
            psR = psum(ph, "psR", [128, 512], F32)
            tq, t_psC, t_psR, t_R, t_Cum = T(), T(), T(), T(), T()
            kb.dma("pool", trs_b[:], trs_d[:], tq)
            kb.dma("sp", thr[:], thr_d[:], tq)
            kb.dma("sp", hp[:], hp_d[:], tq)
            kb.op("pool", lambda e: e.memset(ones_b[:], 1.0), writes=[T()])
            t_ob = T()
            kb.op("dve", lambda e: e.memset(R[:], 0.0), writes=[t_R])
            kb.op("dve", lambda e: e.tensor_tensor(OHs[:], OH1a[:], OH2a[:], ALU.add), reads=[t_rt], writes=[t_ob])
            kb.op("dve", lambda e: e.tensor_copy(OHsb[:], OHs[:]), rw=[t_ob])
            kb.barrier()
            for ti in range(NS):
                kb.op("pe", lambda e, ti=ti: e.matmul(psC[:, 0:NEXP], trs_b[:], OHsb[:, ti, :], start=True, stop=True), reads=[t_ob, tq], writes=[t_psC])
                kb.op("pe", lambda e, ti=ti: e.matmul(psR[:, 0:NEXP], ones_b[:], OHsb[:, ti, :], start=True, stop=True), reads=[t_ob], writes=[t_psR])
                kb.op("dve", lambda e, ti=ti: e.tensor_tensor(Cum[:, ti, :], psC[:, 0:NEXP], R[:], ALU.add), reads=[t_psC, t_R], rw=[t_Cum])
                kb.op("dve", lambda e: e.tensor_tensor(R[:], R[:], psR[:, 0:NEXP], ALU.add), reads=[t_psR], rw=[t_R])
            tz = T()
            kb.op("dve", lambda e: e.tensor_tensor(prod[:], OH1a[:], Cum[:], ALU.mult), reads=[t_rt, t_Cum], writes=[tz])
            kb.op("dve", lambda e: e.tensor_reduce(rank1[:], prod[:], AX.X, ALU.add), rw=[tz])
            kb.op("dve", lambda e: e.tensor_tensor(prod[:], OH2a[:], Cum[:], ALU.mult), reads=[t_rt, t_Cum], rw=[tz])
            kb.op("dve", lambda e: e.tensor_reduce(rank2[:], prod[:], AX.X, ALU.add), rw=[tz])
            kb.op("dve", lambda e: e.tensor_scalar(ci[:], R[:], 511.0, None, ALU.add), reads=[t_R], rw=[tz])
            kb.op("dve", lambda e: e.tensor_scalar(ci[:], ci[:], 9, None, ALU.arith_shift_right), rw=[tz])
            kb.op("dve", lambda e: e.tensor_scalar(ci[:], ci[:], 9, None, ALU.logical_shift_left), rw=[tz])
            kb.op("dve", lambda e: e.tensor_copy(padf[:], ci[:]), rw=[tz])
            kb.op("dve", lambda e: e.tensor_tensor_scan(pend[:], ones_f[:, 0:NEXP], padf[:], 0.0, ALU.mult, ALU.add), reads=[t_ones], rw=[tz])
            kb.op("dve", lambda e: e.tensor_tensor(pstart[:], pend[:], padf[:], ALU.subtract), rw=[tz])
            for (OHx, rk, dsti) in ((OH1a, rank1, dest1i), (OH2a, rank2, dest2i)):
                kb.op("dve", lambda e, OHx=OHx: e.tensor_tensor(prod[:], OHx[:], pstart[:].unsqueeze(1).to_broadcast([128, NS, NEXP]), ALU.mult),
                      reads=[t_rt], rw=[tz])
                kb.op("dve", lambda e: e.tensor_reduce(base[:], prod[:], AX.X, ALU.add), rw=[tz])
                kb.op("dve", lambda e, rk=rk, dsti=dsti: e.tensor_tensor(dsti[:], base[:], rk[:], ALU.add), reads=[tz], rw=[t_rt])
            kb.op("dve", lambda e: e.tensor_tensor(T3[:], pend[:].unsqueeze(1).to_broadcast([128, NBLK, NEXP]),
                                                   thr[:].unsqueeze(2).to_broadcast([128, NBLK, NEXP]), ALU.is_le), reads=[tq], rw=[tz])
            kb.op("dve", lambda e: e.tensor_reduce(be[:], T3[:], AX.X, ALU.add), rw=[tz])
            kb.op("dve", lambda e: e.tensor_scalar(be[:], be[:], float(NEXP - 1), 256.0, ALU.min, ALU.mult), rw=[tz])
            kb.op("dve", lambda e: e.tensor_tensor(idxw[:], be[:].unsqueeze(2).to_broadcast([128, NBLK, 2]),
                                                   hp[:].unsqueeze(1).to_broadcast([128, NBLK, 2]), ALU.add), reads=[tz, tq], rw=[t_rt])
            kb.barrier()

        if stop <= 5:
            return nc
        with ExitStack() as ph:
            wg = [sbuf(ph, "wg%d" % i, [128, 8 * DEXP], BF16) for i in range(2)]
            wu = [sbuf(ph, "wu%d" % i, [128, 8 * DEXP], BF16) for i in range(2)]
            wd = [sbuf(ph, "wd%d" % i, [128, 4 * D], BF16) for i in range(2)]
            xblk = [sbuf(ph, "xblk%d" % i, [128, 4, D], BF16) for i in range(2)]
            xT = sbuf(ph, "xT", [128, 8, 512], BF16)
            sg = [sbuf(ph, "sg%d" % i, [128, 512], BF16) for i in range(2)]
            actT = sbuf(ph, "actT", [128, 4, 512], BF16)
            ybuf = [sbuf(ph, "ybuf%d" % i, [128, 4, D], F32) for i in range(2)]
            x1t = [sbuf(ph, "x1t%d" % i, [128, D], F32) for i in range(4)]
            xs = [xblk[k][:, j, :] for k in range(2) for j in range(4)]
            y1 = [ybuf[0][:, j, :] for j in range(4)]
            y2 = [ybuf[1][:, j, :] for j in range(4)]
            gfB = sbuf(ph, "gfB", [128, D], F32)
            accf = sbuf(ph, "accf", [128, D], F32)
            junk = sbuf(ph, "junkE", [128, D], BF16)
            ssq = sbuf(ph, "ssqE", [128, 1], F32)
            ot = [sbuf(ph, "ot%d" % i, [128, D], F32) for i in range(2)]
            pT = [psum(ph, "pTE%d" % i, [128, 1024], BF16) for i in range(2)]
            pgt = [psum(ph, "pgt%d" % i, [128, 512], F32) for i in range(2)]
            put = [psum(ph, "put%d" % i, [128, 512], F32) for i in range(2)]
            py = [psum(ph, "py%d" % i, [128, 512], F32) for i in range(2)]
            t_zt, t_xs, t_wE, t_xb, t_xT = T(), [T() for _ in range(8)], [T(), T()], [T(), T()], T()
            t_sg, t_actT, t_yb, t_y1, t_y2, t_x1t = [T(), T()], T(), [T(), T()], [T() for _ in range(4)], [T() for _ in range(4)], [T() for _ in range(4)]
            t_gf, t_accf, t_junk, t_ssq, t_ot = T(), T(), T(), T(), [T(), T()]
            t_pT, t_pgt, t_put, t_py = [T(), T()], [T(), T()], [T(), T()], [T(), T()]
            kb.dma("sp", gfB[:], gfin.to_broadcast([128, D]), t_gf)
            XNs = XN.rearrange("(n p) d -> n p d", p=128)
            for ti in range(NS):
                b_ = ti % 8
                kb.dma("sp", xs[b_][:], XNs[ti], t_xs[b_], src=t_XN)
                for dsti in (dest1i, dest2i):
                    kb.dma("pool", None, None, t_XP, src=t_xs[b_], store=True, extra=[t_rt], waw=True,
                           fn=lambda e, dsti=dsti, ti=ti, b_=b_: e.indirect_dma_start(
                               out=XP[:, :], out_offset=bass.IndirectOffsetOnAxis(ap=dsti[:, ti:ti + 1], axis=0),
                               in_=xs[b_][:], in_offset=None))
            kb.barrier()
            XPb = XP.rearrange("(n s p) d -> n p s d", s=4, p=128)
            YPb = YP.rearrange("(n s p) d -> n p s d", s=4, p=128)
            cP, cY = [0], [0]

            def loadWb(b):
                bf = b % 2
                for (wt, wsrc) in ((wg, w_eg), (wu, w_eu), (wd, w_ed)):
                    for hf in range(2):
                        kb.dma("pool", None, None, t_wE[bf], extra=[t_rt],
                               fn=lambda e, wt=wt, wsrc=wsrc, hf=hf, bf=bf, b=b: e.indirect_dma_start(
                                   out=wt[bf][:, hf * 2048:(hf + 1) * 2048], out_offset=None, in_=wsrc[:, :],
                                   in_offset=bass.IndirectOffsetOnAxis(ap=idxw[:, b, hf:hf + 1], axis=0)))
                kb.dma("sp", xblk[bf][:], XPb[b], t_xb[bf], src=t_XP)

            loadWb(0)
            for b in range(NBLK):
                bf = b % 2
                if b + 1 < NBLK:
                    loadWb(b + 1)
                wgv = wg[bf][:].rearrange("p (kc f) -> p kc f", kc=8)
                wuv = wu[bf][:].rearrange("p (kc f) -> p kc f", kc=8)
                wdv = wd[bf][:].rearrange("p (fc d) -> p fc d", fc=4)
                for kc in range(8):
                    pb = kc % 2
                    for s in range(4):
                        kb.op("pe", lambda e, kc=kc, s=s, pb=pb, bf=bf: e.transpose(pT[pb][:, s * 128:(s + 1) * 128], xblk[bf][:, s, kc * 128:(kc + 1) * 128], ident_b[:]),
                              reads=[t_xb[bf], t_const], writes=[t_pT[pb]])
                    if kc % 2 == 0:
                        kb.op("act", lambda e, kc=kc, pb=pb: e.activation(xT[:, kc, :], pT[pb][:, 0:512], AF.Identity, scale=a2[:, kc:kc + 1], bias=b2[:, kc:kc + 1]),
                              reads=[t_pT[pb], t_mod], pw=[t_xT])
                    else:
                        kb.op("dve", lambda e, kc=kc, pb=pb: e.tensor_scalar(xT[:, kc, :], pT[pb][:, 0:512], a2[:, kc:kc + 1], b2[:, kc:kc + 1], ALU.mult, ALU.add),
                              reads=[t_pT[pb], t_mod], pw=[t_xT])
                for fc in range(4):
                    pb = cP[0] % 2
                    cP[0] += 1
                    for kc in range(8):
                        kb.op("pe", lambda e, kc=kc, fc=fc, pb=pb, wgv=wgv: e.matmul(pgt[pb][:, :], wgv[:, kc, fc * 128:(fc + 1) * 128], xT[:, kc, :],
                                                                                  start=(kc == 0), stop=(kc == 7)),
                              reads=[t_wE[bf], t_xT], writes=[t_pgt[pb]])
                    for kc in range(8):
                        kb.op("pe", lambda e, kc=kc, fc=fc, pb=pb, wuv=wuv: e.matmul(put[pb][:, :], wuv[:, kc, fc * 128:(fc + 1) * 128], xT[:, kc, :],
                                                                                  start=(kc == 0), stop=(kc == 7)),
                              reads=[t_wE[bf], t_xT], writes=[t_put[pb]])
                    kb.op("act", lambda e, pb=pb: e.activation(sg[pb][:], pgt[pb][:, :], AF.Silu), reads=[t_pgt[pb]], writes=[t_sg[pb]])
                    kb.op("dve", lambda e, pb=pb, fc=fc: e.tensor_tensor(actT[:, fc, :], sg[pb][:], put[pb][:, :], ALU.mult),
                          reads=[t_sg[pb], t_put[pb]], pw=[t_actT])
                for s in range(4):
                    for hf in range(2):
                        yb = cY[0] % 2
                        cY[0] += 1
                        for fc in range(4):
                            kb.op("pe", lambda e, fc=fc, s=s, hf=hf, yb=yb, wdv=wdv: e.matmul(py[yb][:, :], actT[:, fc, s * 128:(s + 1) * 128],
                                                                                           wdv[:, fc, hf * 512:(hf + 1) * 512], start=(fc == 0), stop=(fc == 3)),
                                  reads=[t_actT, t_wE[bf]], writes=[t_py[yb]])
                        if hf == 0:
                            kb.op("act", lambda e, yb=yb, s=s, hf=hf, bf=bf: e.activation(ybuf[bf][:, s, hf * 512:(hf + 1) * 512], py[yb][:, :], AF.Identity),
                                  reads=[t_py[yb]], pw=[t_yb[bf]])
                        else:
                            kb.op("dve", lambda e, yb=yb, s=s, hf=hf, bf=bf: e.tensor_copy(ybuf[bf][:, s, hf * 512:(hf + 1) * 512], py[yb][:, :]),
                                  reads=[t_py[yb]], pw=[t_yb[bf]])
                kb.dma("sp", YPb[b], ybuf[bf][:], t_YP, src=t_yb[bf], store=True)
            kb.barrier()
            X1s = X1.rearrange("(n p) d -> n p d", p=128)
            outs = out.rearrange("(n p) d -> n p d", p=128)
            for ti in range(NS):
                ob = ti % 4
                o2 = ti % 2
                kb.dma("pool", None, None, t_y1[ob], src=t_YP, extra=[t_rt],
                       fn=lambda e, ti=ti, ob=ob: e.indirect_dma_start(out=y1[ob][:], out_offset=None, in_=YP[:, :],
                                                                      in_offset=bass.IndirectOffsetOnAxis(ap=dest1i[:, ti:ti + 1], axis=0)))
                kb.dma("pool", None, None, t_y2[ob], src=t_YP, extra=[t_rt],
                       fn=lambda e, ti=ti, ob=ob: e.indirect_dma_start(out=y2[ob][:], out_offset=None, in_=YP[:, :],
                                                                      in_offset=bass.IndirectOffsetOnAxis(ap=dest2i[:, ti:ti + 1], axis=0)))
                kb.dma("sp", x1t[ob][:], X1s[ti], t_x1t[ob], src=t_X1)
                kb.op("act", lambda e, ti=ti, ob=ob: e.activation(accf[:], y1[ob][:], AF.Identity, scale=w1a[:, ti:ti + 1]), reads=[t_y1[ob], t_rt], writes=[t_accf])
                kb.op("dve", lambda e, ti=ti, ob=ob: e.scalar_tensor_tensor(accf[:], y2[ob][:], w2a[:, ti:ti + 1], accf[:], ALU.mult, ALU.add),
                      reads=[t_y2[ob], t_rt], rw=[t_accf])
                kb.op("dve", lambda e: e.tensor_tensor(accf[:], accf[:], gate2B[:], ALU.mult), reads=[t_mod], rw=[t_accf])
                kb.op("dve", lambda e, ob=ob: e.tensor_tensor(accf[:], accf[:], x1t[ob][:], ALU.add), reads=[t_x1t[ob]], rw=[t_accf])
                kb.op("act", lambda e: e.activation(junk[:], accf[:], AF.Square, accum_out=ssq[:]), reads=[t_accf], writes=[t_junk, t_ssq])
                kb.op("act", lambda e: e.activation(ssq[:], ssq[:], AF.Sqrt, scale=1.0 / D, bias=EPS), rw=[t_ssq])
                kb.op("dve", lambda e: e.reciprocal(ssq[:], ssq[:]), rw=[t_ssq])
                kb.op("dve", lambda e, o2=o2: e.scalar_tensor_tensor(ot[o2][:], accf[:], ssq[:, 0:1], gfB[:], ALU.mult, ALU.mult),
                      reads=[t_accf, t_ssq, t_gf], writes=[t_ot[o2]])
                kb.dma("sp", outs[ti], ot[o2][:], t_out, src=t_ot[o2], store=True)
            kb.barrier()
    return nc


def _consts():
    ident = np.eye(128, dtype=np.float32)
    tri = np.triu(np.ones((128, 128), np.float32))
    sel4 = np.zeros((4, 4, 128), np.float32)
    for h in range(4):
        sel4[h, h, :] = 1.0
    trs = np.triu(np.ones((128, 128), np.float32), k=1)
    hp = np.stack([np.arange(128, dtype=np.float32), 128.0 + np.arange(128, dtype=np.float32)], axis=1)
    return ident, tri, sel4, trs, hp


def prep_core_inputs(inp, b):
    f = np.float32
    ident, tri, sel4, trs, hp = _consts()
    S = np.asarray(inp["x"]).shape[1]
    nblk = (2 * S) // 512 + NEXP
    thr = np.ascontiguousarray(np.broadcast_to(512.0 * np.arange(nblk, dtype=np.float32), (128, nblk)))

    def pl(v):
        return np.ascontiguousarray(np.asarray(v, f).reshape(8, 128).T)

    bg = np.asarray(inp["b_gates"], f)[0]
    wconv = np.asarray(inp["w_conv"], f)[0]
    m = {
        "x": np.ascontiguousarray(np.asarray(inp["x"], f)[b]),
        "c_l": pl(np.asarray(inp["c"], f)[b]),
        "w_ada": np.ascontiguousarray(np.asarray(inp["w_ada"], f)[0]),
        "b_ada": np.ascontiguousarray(np.asarray(inp["b_ada"], f)[0].reshape(1, -1)),
        "g1_l": pl(inp["g_norm1"][0]),
        "g2_l": pl(inp["g_norm2"][0]),
        "gfin": np.ascontiguousarray(np.asarray(inp["g_final"], f).reshape(1, -1)),
        "w_in": np.ascontiguousarray(np.asarray(inp["w_in"], f)[0]),
        "wconv_l": np.ascontiguousarray(wconv.reshape(4, 8, 128).transpose(2, 1, 0)),
        "bconv_l": pl(inp["b_conv"][0]),
        "bgi": np.ascontiguousarray(bg[0:4].reshape(4, 1)),
        "bgf": np.ascontiguousarray(bg[4:8].reshape(4, 1)),
        "gml": np.ascontiguousarray(np.asarray(inp["g_mlstm_head"], f)[0].reshape(1, -1)),
        "w_out": np.ascontiguousarray(np.asarray(inp["w_out"], f)[0]),
        "w_r": np.ascontiguousarray(np.concatenate([np.asarray(inp["w_router_group"], f)[0],
                                                    np.asarray(inp["w_router_expert"], f)[0]], axis=1)),
        "b_r": np.ascontiguousarray(np.concatenate([np.asarray(inp["b_router_group"], f)[0],
                                                    np.asarray(inp["b_router_expert"], f)[0]]).reshape(1, -1)),
        "ident": ident, "tri": tri, "sel4": sel4, "trs": trs, "hp": hp, "thr": thr,
    }
    return m


def prep_shared(inp):
    f = np.float32
    def lay(w, n):
        C = w.shape[2]
        return np.ascontiguousarray(w.reshape(NEXP, 2, n, 128, C).transpose(0, 1, 3, 2, 4).reshape(NEXP * 2 * 128, n * C))
    return {
        "w_eg": lay(np.asarray(inp["w_expert_gate"], f)[0], 4),
        "w_eu": lay(np.asarray(inp["w_expert_up"], f)[0], 4),
        "w_ed": lay(np.asarray(inp["w_expert_down"], f)[0], 2),
    }


def kernel(**inputs):
    B, S, _ = inputs["x"].shape
    nc = build(S)
    shared = prep_shared(inputs)
    in_maps = [dict(prep_core_inputs(inputs, b), **shared) for b in range(B)]
    res = run_bass_kernel_spmd(nc, in_maps, core_ids=list(range(B)))
    return np.stack([np.asarray(r["out"], np.float32) for r in res.results], axis=0)
```
